# Optimizing a Trainium2 kernel written in Bass

```python
import math
import jax, jax.numpy as jnp
from jax import lax
import numpy as np

D_MODEL = 2048
BATCH = 2
SEQ = 16384
DEPTH = 2

GRID_W = 64
N_HEADS = 16
HEAD_DIM = 64
ATT_W = N_HEADS * HEAD_DIM
KH_MAX = 8
KW = 16
CONV_W = 1024
CONV_K = 31
PLE_DIM = 256
N_EXPERTS = 16
CAP_FACTOR = 2
D_EXPERT = 2048
N_IN = 3 * ATT_W + 2 * CONV_W + 2 * D_MODEL
EPS = 1e-6

kernel_name = "hybrid_natten_conformer_ecmoe_block"


def _rms(x):
    xf = x.astype(jnp.float32)
    return xf * lax.rsqrt(jnp.mean(xf * xf, axis=-1, keepdims=True) + EPS)


def rmsnorm(x, g):
    return (_rms(x) * g.astype(jnp.float32)).astype(x.dtype)


def layernorm(x, g, b):
    xf = x.astype(jnp.float32)
    mu = jnp.mean(xf, axis=-1, keepdims=True)
    var = jnp.mean(jnp.square(xf - mu), axis=-1, keepdims=True)
    y = (xf - mu) * lax.rsqrt(var + EPS) * g.astype(jnp.float32) + b.astype(jnp.float32)
    return y.astype(x.dtype)


def neighbourhood_attention(q, k, v, q_g, k_g, rpb):
    B, S, _ = q.shape
    rows = S // GRID_W
    kh = min(KH_MAX, rows)
    shp = (B, rows, GRID_W, N_HEADS, HEAD_DIM)
    q = rmsnorm(q.reshape(shp), q_g)
    k = rmsnorm(k.reshape(shp), k_g)
    v = v.reshape(shp)
    scale = 1.0 / math.sqrt(HEAD_DIM)

    cols = np.arange(GRID_W)
    col_start = np.clip(cols - KW // 2, 0, GRID_W - KW)
    col_idx = col_start[:, None] + np.arange(KW)[None, :]
    dc = col_idx - cols[:, None]
    col_ix = jnp.asarray(dc + KW - 1)[:, None, :]
    col_idx = jnp.asarray(col_idx)

    def one_row(args):
        q_r, r = args
        start = jnp.clip(r - kh // 2, 0, rows - kh)
        k_rows = lax.dynamic_slice_in_dim(k, start, kh, axis=1)
        v_rows = lax.dynamic_slice_in_dim(v, start, kh, axis=1)
        k_win = k_rows[:, :, col_idx]
        v_win = v_rows[:, :, col_idx]
        s = jnp.einsum('bqhd,biqjhd->bhqij', q_r, k_win).astype(jnp.float32) * scale
        dr = start + jnp.arange(kh) - r
        row_ix = (dr + KH_MAX - 1)[None, :, None]
        bias = rpb[:, row_ix, col_ix].astype(jnp.float32)
        s = s + bias[None]
        pr = jax.nn.softmax(s.reshape(B, N_HEADS, GRID_W, kh * KW), axis=-1)
        pr = pr.reshape(B, N_HEADS, GRID_W, kh, KW).astype(v.dtype)
        return jnp.einsum('bhqij,biqjhd->bqhd', pr, v_win)

    q_rows = jnp.transpose(q, (1, 0, 2, 3, 4))
    out = lax.map(one_row, (q_rows, jnp.arange(rows)))
    return jnp.transpose(out, (1, 0, 2, 3, 4)).reshape(B, S, ATT_W)


def conformer_conv(a, b, w_dw, b_dw, ln_g, ln_b):
    u = a * jax.nn.sigmoid(b)
    u = lax.conv_general_dilated(
        u, w_dw[:, None, :].astype(u.dtype), window_strides=(1,),
        padding=[(CONV_K // 2, CONV_K // 2)],
        dimension_numbers=('NWC', 'WIO', 'NWC'),
        feature_group_count=CONV_W) + b_dw
    u = layernorm(u, ln_g, ln_b)
    return jax.nn.silu(u)


def expert_choice_moe(h, w_router, w1, w3, w2):
    B, S, _ = h.shape
    cap = CAP_FACTOR * S // N_EXPERTS
    logits = jnp.einsum('bsd,de->bse', h, w_router).astype(jnp.float32)
    aff = jax.nn.softmax(logits, axis=-1)
    gate, idx = lax.top_k(jnp.transpose(aff, (0, 2, 1)), cap)
    bidx = jnp.arange(B)[:, None, None]
    xe = h[bidx, idx]
    hid = jax.nn.silu(jnp.einsum('becd,edf->becf', xe, w1)) * jnp.einsum('becd,edf->becf', xe, w3)
    ye = jnp.einsum('becf,efd->becd', hid, w2) * gate[..., None].astype(h.dtype)
    return jnp.zeros_like(h).at[bidx, idx].add(ye)


def setup_inputs(seed: int = 0) -> dict:
    key = jax.random.key(seed)
    ks = jax.random.split(key, 24)
    L, D, C, E, F = DEPTH, D_MODEL, CONV_W, N_EXPERTS, D_EXPERT
    nrm = lambda k, shape, s: jax.random.normal(k, shape, jnp.float32) * s
    gain = lambda k, shape: 1.0 + 0.05 * jax.random.normal(k, shape, jnp.float32)
    return {
        "x": nrm(ks[0], (BATCH, SEQ, D), 1.0),
        "p": nrm(ks[1], (DEPTH, BATCH, SEQ, PLE_DIM), 1.0),
        "norm1_g": gain(ks[2], (L, D)),
        "w_in": nrm(ks[3], (L, D, N_IN), D ** -0.5),
        "q_norm_g": gain(ks[4], (L, HEAD_DIM)),
        "k_norm_g": gain(ks[5], (L, HEAD_DIM)),
        "rpb": nrm(ks[6], (L, N_HEADS, 2 * KH_MAX - 1, 2 * KW - 1), 0.1),
        "w_dw": nrm(ks[7], (L, CONV_K, C), CONV_K ** -0.5),
        "b_dw": nrm(ks[8], (L, C), 0.02),
        "conv_ln_g": gain(ks[9], (L, C)),
        "conv_ln_b": nrm(ks[10], (L, C), 0.02),
        "w_att_o": nrm(ks[11], (L, ATT_W, D), ATT_W ** -0.5),
        "w_conv_o": nrm(ks[12], (L, C, D), C ** -0.5),
        "w_out": nrm(ks[13], (L, D, D), D ** -0.5),
        "norm2_g": gain(ks[14], (L, D)),
        "w_router": nrm(ks[15], (L, D, E), D ** -0.5),
        "w1": nrm(ks[16], (L, E, D, F), D ** -0.5),
        "w3": nrm(ks[17], (L, E, D, F), D ** -0.5),
        "w2": nrm(ks[18], (L, E, F, D), F ** -0.5),
        "w_pe": nrm(ks[19], (L, PLE_DIM, D), PLE_DIM ** -0.5),
        "pe_norm_g": gain(ks[20], (L, D)),
        "w_pg": nrm(ks[21], (L, D, D), D ** -0.5),
    }


def reference(x, p, norm1_g, w_in, q_norm_g, k_norm_g, rpb, w_dw, b_dw, conv_ln_g,
              conv_ln_b, w_att_o, w_conv_o, w_out, norm2_g, w_router, w1, w3, w2,
              w_pe, pe_norm_g, w_pg):
    o_k, o_v = ATT_W, 2 * ATT_W
    o_a, o_b = 3 * ATT_W, 3 * ATT_W + CONV_W
    o_ga = 3 * ATT_W + 2 * CONV_W
    o_gc = o_ga + D_MODEL
    for i in range(DEPTH):
        h = rmsnorm(x, norm1_g[i])
        u = jnp.einsum('bsd,dn->bsn', h, w_in[i])
        q, k, v = u[..., :o_k], u[..., o_k:o_v], u[..., o_v:o_a]
        ca, cb = u[..., o_a:o_b], u[..., o_b:o_ga]
        g_att = jax.nn.sigmoid(u[..., o_ga:o_gc])
        g_conv = jax.nn.sigmoid(u[..., o_gc:])
        y_att = neighbourhood_attention(q, k, v, q_norm_g[i], k_norm_g[i], rpb[i]) @ w_att_o[i]
        y_conv = conformer_conv(ca, cb, w_dw[i], b_dw[i], conv_ln_g[i], conv_ln_b[i]) @ w_conv_o[i]
        x = x + (g_att * y_att + g_conv * y_conv) @ w_out[i]
        x = x + expert_choice_moe(rmsnorm(x, norm2_g[i]), w_router[i], w1[i], w3[i], w2[i])
        gate = jax.nn.sigmoid(jnp.einsum('bsd,de->bse', _rms(x).astype(x.dtype), w_pg[i]))
        pe = rmsnorm(jnp.einsum('bsk,kd->bsd', p[i], w_pe[i]), pe_norm_g[i])
        x = x + gate * pe
    return x
```

```python
from contextlib import ExitStack

import numpy as np
import concourse.bass as bass
import concourse.mybir as mybir
from concourse.bass_utils import run_bass_kernel_spmd

F32 = mybir.dt.float32
BF16 = mybir.dt.bfloat16
I32 = mybir.dt.int32
U32 = mybir.dt.uint32
AF = mybir.ActivationFunctionType
ALU = mybir.AluOpType
AX = mybir.AxisListType

D = 2048
T = 4096
HALO = 256
TE = T + 2 * HALO
NIN = 9216
EPS = 1e-6
NL = 2
NCORES = 8
CAP = 2048
NEG = -30000.0

ATT_RNG = [(0, 1), (0, 3), (0, 7), (0, 7), (0, 7), (0, 7), (5, 7), (7, 7)]
ATT_ORDER = [2, 3, 4, 5, 0, 1, 6, 7]
ATT_COLS = sum((b - a + 1) * 64 for a, b in ATT_RNG)


class Buf:
    __slots__ = ("t", "w", "r")

    def __init__(self, t):
        self.t = t
        self.w = None
        self.r = []

    def __getitem__(self, k):
        return self.t[k]


class Sched:
    def __init__(self, nc, n_dma_sems=8):
        self.nc = nc
        self.eng = {"pe": nc.tensor, "act": nc.scalar, "dve": nc.vector, "pool": nc.gpsimd, "sp": nc.sync}
        self.sem, self.cnt, self._ctx = {}, {}, []
        for e in self.eng:
            cm = nc.semaphore("s_" + e)
            self.sem[e] = cm.__enter__()
            self._ctx.append(cm)
            self.cnt[e] = 0
        self.dsem, self.dcnt, self.dnext, self.dtok = {}, {}, {}, {}
        for q in ("sp", "pool", "act"):
            self.dsem[q] = []
            for i in range(n_dma_sems):
                cm = nc.semaphore("d_%s%d" % (q, i))
                self.dsem[q].append(cm.__enter__())
                self._ctx.append(cm)
            self.dcnt[q] = [0] * n_dma_sems
            self.dtok[q] = [None] * n_dma_sems
            self.dnext[q] = 0
        self.seen = {e: {} for e in self.eng}

    def close(self):
        for cm in reversed(self._ctx):
            cm.__exit__(None, None, None)

    def _wait(self, e, tok):
        if tok is None:
            return
        s, v = tok
        k = id(s)
        if self.seen[e].get(k, 0) >= v:
            return
        self.seen[e][k] = v
        self.eng[e].wait_ge(s, v)

    def _deps(self, e, reads, writes):
        for b in reads:
            self._wait(e, b.w)
        for b in writes:
            self._wait(e, b.w)
            for t in b.r:
                self._wait(e, t)

    def _commit(self, tok, reads, writes):
        for b in reads:
            b.r.append(tok)
            if len(b.r) > 32:
                b.r = b.r[-32:]
        for b in writes:
            b.w = tok
            b.r = []

    def op(self, e, fn, reads=(), writes=()):
        self._deps(e, reads, writes)
        inst = fn(self.eng[e])
        self.cnt[e] += 1
        inst.then_inc(self.sem[e], 1)
        tok = (self.sem[e], self.cnt[e])
        self._commit(tok, reads, writes)
        return tok

    def dma(self, q, fn, reads=(), writes=()):
        i = self.dnext[q]
        self.dnext[q] = (i + 1) % len(self.dsem[q])
        self._wait(q, self.dtok[q][i])
        self._deps(q, reads, writes)
        inst = fn(self.eng[q])
        self.dcnt[q][i] += 16
        inst.then_inc(self.dsem[q][i], 16)
        tok = (self.dsem[q][i], self.dcnt[q][i])
        self.dtok[q][i] = tok
        self._commit(tok, reads, writes)
        return tok

    def coll(self, fn, reads=(), writes=()):
        if "cc" not in self.sem:
            cm = self.nc.semaphore("s_cc")
            self.sem["cc"] = cm.__enter__()
            self._ctx.append(cm)
            self.cnt["cc"] = 0
        self._deps("pool", reads, writes)
        inst = fn(self.eng["pool"])
        self.cnt["cc"] += 1
        inst.then_inc(self.sem["cc"])
        tok = (self.sem["cc"], self.cnt["cc"])
        self._commit(tok, reads, writes)
        return tok

    def barrier(self, engines=None):
        toks = []
        if self.cnt.get("cc"):
            toks.append((self.sem["cc"], self.cnt["cc"]))
        for e in self.eng:
            if self.cnt[e]:
                toks.append((self.sem[e], self.cnt[e]))
        for q in self.dsem:
            for t in self.dtok[q]:
                if t is not None:
                    toks.append(t)
        for e in (engines or self.eng):
            for t in toks:
                self._wait(e, t)


class Ctx:
    pass


def chain(S, e, fns, reads, writes):
    for f in fns:
        S.op(e, f, reads=reads, writes=writes)


_UID = [0]


def _uname(name):
    _UID[0] += 1
    return "%s_u%d" % (name, _UID[0])


def sb(nc, st, name, shape, dt):
    return Buf(st.enter_context(nc.sbuf_tensor(_uname(name), shape, dt)))


def ps(nc, st, name, shape, dt=F32):
    return Buf(st.enter_context(nc.psum_tensor(_uname(name), shape, dt)))


def fm(ap):
    return ap.rearrange("(kc p) t -> p kc t", p=128)


_CAST_BUF = Buf(None)


def op_cast(S, src, dst, R, C):
    c = 2048
    while C % c:
        c //= 2
    per_row = C // c
    rows_per = max(1, 4096 // per_row)
    rows_per = max(1, 2048 // per_row)
    for r0 in range(0, R, rows_per):
        r1 = min(R, r0 + rows_per)
        S.dma("pool", lambda e, r0=r0, r1=r1: e.dma_start(
            out=dst[r0:r1, :].rearrange("r (a c) -> r a c", c=c),
            in_=src[r0:r1, :].rearrange("r (a c) -> r a c", c=c)), writes=[_CAST_BUF])


def op_norm_T(S, nc, K, xsrc, ntok, gtile_ap, hT_dst, tag):
    with ExitStack() as st:
        xt = [sb(nc, st, "%s_x%d" % (tag, i), [128, D], F32) for i in range(2)]
        junk = sb(nc, st, tag + "_junk", [128, D], F32)
        xn = [sb(nc, st, "%s_xn%d" % (tag, i), [128, D], BF16) for i in range(2)]
        ss = [sb(nc, st, "%s_ss%d" % (tag, i), [128, 2], F32) for i in range(2)]
        hTt = [sb(nc, st, "%s_hT%d" % (tag, i), [128, 16, 512], BF16) for i in range(2)]
        pT = [ps(nc, st, "%s_pT%d" % (tag, i), [128, D], BF16) for i in range(2)]
        g = None
        if gtile_ap is not None:
            g = sb(nc, st, tag + "_g", [128, D], F32)
            S.dma("sp", lambda e: e.dma_start(out=g[:], in_=gtile_ap), writes=[g])
        n = 0
        for grp in range(ntok // 512):
            hb = hTt[grp % 2]
            for s in range(4):
                i = n % 2
                t0 = grp * 512 + s * 128
                S.dma("sp", lambda e, i=i, t0=t0: e.dma_start(out=xt[i][:], in_=xsrc[t0:t0 + 128, :]), writes=[xt[i]])
                S.op("act", lambda e, i=i: e.activation(out=junk[:], in_=xt[i][:], func=AF.Square), reads=[xt[i]], writes=[junk])
                S.op("dve", lambda e, i=i: e.tensor_reduce(out=ss[i][:, 0:1], in_=junk[:], axis=AX.X, op=ALU.add), reads=[junk], writes=[ss[i]])

                S.op("act", lambda e, i=i: e.activation(out=ss[i][:, 1:2], in_=ss[i][:, 0:1], func=AF.Sqrt, scale=1.0 / D, bias=K.epsc[:, 0:1]),
                     reads=[ss[i], K.epsc], writes=[ss[i]])
                S.op("dve", lambda e, i=i: e.reciprocal(out=ss[i][:, 1:2], in_=ss[i][:, 1:2]), reads=[ss[i]], writes=[ss[i]])
                if g is not None:
                    S.op("dve", lambda e, i=i: e.scalar_tensor_tensor(out=xn[i][:], in0=xt[i][:], scalar=ss[i][:, 1:2], in1=g[:],
                                                                      op0=ALU.mult, op1=ALU.mult),
                         reads=[xt[i], ss[i], g], writes=[xn[i]])
                else:
                    S.op("dve", lambda e, i=i: e.tensor_scalar(out=xn[i][:], in0=xt[i][:], scalar1=ss[i][:, 1:2], scalar2=None, op0=ALU.mult),
                         reads=[xt[i], ss[i]], writes=[xn[i]])

                def tr(e, i=i):
                    for kc in range(16):
                        ins = e.transpose(out=pT[i][:, kc * 128:(kc + 1) * 128], in_=xn[i][:, kc * 128:(kc + 1) * 128], identity=K.identb[:])
                    return ins
                S.op("pe", tr, reads=[xn[i], K.identb], writes=[pT[i]])
                S.op("act", lambda e, i=i, s=s, hb=hb: e.activation(out=hb[:, :, s * 128:(s + 1) * 128],
                                                                    in_=pT[i][:].rearrange("p (k t) -> p k t", k=16), func=AF.Copy),
                     reads=[pT[i]], writes=[hb])
                n += 1
            S.dma("pool", lambda e, hb=hb, grp=grp: e.dma_start(out=fm(hT_dst)[:, :, grp * 512:(grp + 1) * 512], in_=hb[:]), reads=[hb])
        S.barrier()


def op_inproj(S, nc, K, L):
    hT, wb = K.hT, K.wb_in
    TS = 1536
    with ExitStack() as st:
        hTs = sb(nc, st, "ip_hTs", [128, 16, TS], BF16)
        wch = [sb(nc, st, "ip_w%d" % i, [128, 16, 512], BF16) for i in range(4)]
        pa = [ps(nc, st, "ip_pa%d" % i, [128, 512]) for i in range(2)]
        pb = [ps(nc, st, "ip_pb%d" % i, [128, 512]) for i in range(2)]
        pc = [ps(nc, st, "ip_pc%d" % i, [128, 512]) for i in range(2)]
        sq = [sb(nc, st, "ip_sq%d" % i, [128, 512], F32) for i in range(2)]
        rs = [sb(nc, st, "ip_rs%d" % i, [128, 512], F32) for i in range(2)]
        ob = [sb(nc, st, "ip_ob%d" % i, [128, 512], BF16) for i in range(3)]
        of = [sb(nc, st, "ip_of%d" % i, [128, 512], F32) for i in range(2)]
        sg = [sb(nc, st, "ip_sg%d" % i, [128, 512], F32) for i in range(2)]
        wn = [0]

        def loadw(blk):
            w = wch[wn[0] % 4]
            wn[0] += 1
            S.dma("sp", lambda e, w=w, blk=blk: e.dma_start(out=w[:], in_=fm(wb)[:, :, blk * 512:(blk + 1) * 512]), writes=[w])
            return w

        cn = [0]
        for sti in range(TE // TS):
            t_base = sti * TS
            S.dma("sp", lambda e, t_base=t_base: e.dma_start(out=hTs[:], in_=fm(hT)[:, :, t_base:t_base + TS]), writes=[hTs])

            def mm_fm(P, w, j, tt):
                def f(e):
                    for kc in range(16):
                        ins = e.matmul(P[:], w[:, kc, j * 128:(j + 1) * 128], hTs[:, kc, tt * 512:(tt + 1) * 512],
                                       start=(kc == 0), stop=(kc == 15))
                    return ins
                S.op("pe", f, reads=[w, hTs], writes=[P])

            for blk in range(4):
                w = loadw(blk)
                dst = K.qT if blk < 2 else K.kT
                gv = K.qg if blk < 2 else K.kg
                for j in range(4):
                    row0 = ((blk % 2) * 4 + j) * 128
                    for tt in range(TS // 512):
                        c = cn[0] % 2
                        cn[0] += 1
                        A, C = pa[c], pc[c]
                        mm_fm(A, w, j, tt)
                        S.op("act", lambda e, A=A, c=c: e.activation(out=sq[c][:], in_=A[:], func=AF.Square), reads=[A], writes=[sq[c]])
                        S.op("pe", lambda e, C=C, c=c: e.matmul(C[:], K.blk64[:], sq[c][:], start=True, stop=True),
                             reads=[K.blk64, sq[c]], writes=[C])

                        S.op("act", lambda e, C=C, c=c: e.activation(out=rs[c][:], in_=C[:], func=AF.Sqrt, scale=1.0 / 64, bias=K.epsc[:, 0:1]),
                             reads=[C, K.epsc], writes=[rs[c]])
                        S.op("dve", lambda e, c=c: e.reciprocal(out=rs[c][:], in_=rs[c][:]), reads=[rs[c]], writes=[rs[c]])
                        o = ob[cn[0] % 3]
                        S.op("dve", lambda e, A=A, c=c, o=o, gv=gv: e.scalar_tensor_tensor(
                            out=o[:], in0=A[:], scalar=gv[:, 0:1], in1=rs[c][:], op0=ALU.mult, op1=ALU.mult),
                            reads=[A, rs[c], gv], writes=[o])
                        t0 = t_base + tt * 512
                        S.dma("pool", lambda e, o=o, dst=dst, row0=row0, t0=t0: e.dma_start(out=dst[row0:row0 + 128, t0:t0 + 512], in_=o[:]),
                              reads=[o])
            for blk in (4, 5):
                w = loadw(blk)
                for t128 in range(TS // 128):
                    c = cn[0] % 2
                    cn[0] += 1
                    A = pa[c]

                    def f(e, A=A, w=w, t128=t128):
                        for kc in range(16):
                            ins = e.matmul(A[:], hTs[:, kc, t128 * 128:(t128 + 1) * 128], w[:, kc, :], start=(kc == 0), stop=(kc == 15))
                        return ins
                    S.op("pe", f, reads=[w, hTs], writes=[A])
                    o = ob[cn[0] % 3]
                    S.op("act", lambda e, A=A, o=o: e.activation(out=o[:], in_=A[:], func=AF.Copy), reads=[A], writes=[o])
                    t0 = t_base + t128 * 128
                    S.dma("pool", lambda e, o=o, t0=t0, blk=blk: e.dma_start(out=K.v[t0:t0 + 128, (blk - 4) * 512:(blk - 3) * 512], in_=o[:]),
                          reads=[o])
            for u in range(2):
                wa = loadw(6 + u)
                wb_ = loadw(8 + u)
                for j in range(4):
                    row0 = (u * 4 + j) * 128
                    for tt in range(TS // 512):
                        c = cn[0] % 2
                        cn[0] += 1
                        A, B = pa[c], pb[c]
                        mm_fm(A, wa, j, tt)
                        mm_fm(B, wb_, j, tt)
                        S.op("act", lambda e, B=B, c=c: e.activation(out=sg[c][:], in_=B[:], func=AF.Sigmoid), reads=[B], writes=[sg[c]])
                        S.op("dve", lambda e, A=A, c=c: e.tensor_tensor(out=of[c][:], in0=A[:], in1=sg[c][:], op=ALU.mult),
                             reads=[A, sg[c]], writes=[of[c]])
                        t0 = t_base + tt * 512
                        S.dma("pool", lambda e, c=c, row0=row0, t0=t0: e.dma_start(out=K.gluT[row0:row0 + 128, t0:t0 + 512], in_=of[c][:]),
                              reads=[of[c]])
            for blk in range(10, 18):
                w = loadw(blk)
                dst = K.gaT if blk < 14 else K.gcT
                for j in range(4):
                    row0 = (((blk - 10) % 4) * 4 + j) * 128
                    for tt in range(TS // 512):
                        c = cn[0] % 2
                        cn[0] += 1
                        A = pa[c]
                        mm_fm(A, w, j, tt)
                        o = ob[cn[0] % 3]
                        S.op("act", lambda e, A=A, o=o: e.activation(out=o[:], in_=A[:], func=AF.Sigmoid), reads=[A], writes=[o])
                        t0 = t_base + tt * 512
                        S.dma("pool", lambda e, o=o, dst=dst, row0=row0, t0=t0: e.dma_start(out=dst[row0:row0 + 128, t0:t0 + 512], in_=o[:]),
                              reads=[o])
        S.barrier()


def op_attention(S, nc, K, L):
    with ExitStack() as st:
        kth = [sb(nc, st, "at_k%d" % i, [64, TE], BF16) for i in range(2)]
        qth = [sb(nc, st, "at_q%d" % i, [64, T], BF16) for i in range(2)]
        vh = [sb(nc, st, "at_v%d" % i, [128, TE // 128, 64], BF16) for i in range(2)]
        tab = [sb(nc, st, "at_tab%d" % i, [128, 3, ATT_COLS], F32) for i in range(2)]
        sT = [ps(nc, st, "at_s%d" % i, [128, 512]) for i in range(2)]
        pv = [ps(nc, st, "at_pv%d" % i, [64, 512]) for i in range(2)]
        pz = [ps(nc, st, "at_pz%d" % i, [64, 512]) for i in range(2)]
        sf = [sb(nc, st, "at_sf%d" % i, [128, 512], F32) for i in range(2)]
        pe_ = [sb(nc, st, "at_p%d" % i, [128, 512], BF16) for i in range(3)]
        rz = [sb(nc, st, "at_rz%d" % i, [64, 512], F32) for i in range(2)]
        ao = [sb(nc, st, "at_ao%d" % i, [64, 512], BF16) for i in range(2)]
        coff = []
        o = 0
        for a, b in ATT_RNG:
            coff.append(o)
            o += (b - a + 1) * 64
        n = 0
        for h in range(16):
            hb = h % 2
            r0 = h * 64
            S.dma("sp", lambda e, hb=hb, r0=r0: e.dma_start(out=kth[hb][:], in_=K.kT[r0:r0 + 64, :]), writes=[kth[hb]])
            S.dma("sp", lambda e, hb=hb, r0=r0: e.dma_start(out=qth[hb][:], in_=K.qT[r0:r0 + 64, HALO:HALO + T]), writes=[qth[hb]])
            with nc.allow_non_contiguous_dma(reason="per-head V slice"):
                S.dma("sp", lambda e, hb=hb, r0=r0: e.dma_start(out=vh[hb][:], in_=K.v.rearrange("(c p) n -> p c n", p=128)[:, :, r0:r0 + 64]),
                      writes=[vh[hb]])
            S.dma("sp", lambda e, hb=hb, h=h: e.dma_start(out=tab[hb][:], in_=K.att_tab[L, h]), writes=[tab[hb]])
            for blk in range(8):
                slot = 0 if blk == 0 else (2 if blk == 7 else 1)
                PV, PZ = pv[blk % 2], pz[blk % 2]
                first = True
                for oi, ci in enumerate(ATT_ORDER):
                    last = oi == len(ATT_ORDER) - 1
                    a, b = ATT_RNG[ci]
                    ncol = (b - a + 1) * 64
                    q0 = blk * 512 + a * 64
                    kt0 = blk * 512 + ci * 128
                    c = n % 2
                    p3 = pe_[n % 3]
                    n += 1
                    S.op("pe", lambda e, c=c, hb=hb, kt0=kt0, q0=q0, ncol=ncol: e.matmul(
                        sT[c][:, 0:ncol], kth[hb][:, kt0:kt0 + 128], qth[hb][:, q0:q0 + ncol], start=True, stop=True),
                        reads=[kth[hb], qth[hb]], writes=[sT[c]])
                    S.op("dve", lambda e, c=c, hb=hb, slot=slot, ci=ci, ncol=ncol: e.tensor_tensor(
                        out=sf[c][:, 0:ncol], in0=sT[c][:, 0:ncol], in1=tab[hb][:, slot, coff[ci]:coff[ci] + ncol], op=ALU.add),
                        reads=[sT[c], tab[hb]], writes=[sf[c]])
                    S.op("act", lambda e, c=c, p3=p3, ncol=ncol: e.activation(out=p3[:, 0:ncol], in_=sf[c][:, 0:ncol], func=AF.Exp),
                         reads=[sf[c]], writes=[p3])
                    kc = kt0 // 128

                    def pvf(e, p3=p3, hb=hb, kc=kc, a=a, ncol=ncol, first=first, last=last, PV=PV, PZ=PZ):
                        e.matmul(PV[:, a * 64:a * 64 + ncol], vh[hb][:, kc, :], p3[:, 0:ncol], start=first, stop=last, skip_group_check=True)
                        return e.matmul(PZ[:, a * 64:a * 64 + ncol], K.ones_b[:, 0:64], p3[:, 0:ncol], start=first, stop=last,
                                        skip_group_check=True)
                    S.op("pe", pvf, reads=[p3, vh[hb], K.ones_b], writes=[PV, PZ])
                    first = False
                i2 = blk % 2
                S.op("dve", lambda e, i2=i2, PZ=PZ: e.reciprocal(out=rz[i2][:], in_=PZ[:]), reads=[PZ], writes=[rz[i2]])
                S.op("dve", lambda e, i2=i2, PV=PV: e.tensor_tensor(out=ao[i2][:], in0=PV[:], in1=rz[i2][:], op=ALU.mult),
                     reads=[PV, rz[i2]], writes=[ao[i2]])
                S.dma("pool", lambda e, i2=i2, r0=r0, blk=blk: e.dma_start(out=K.attT[r0:r0 + 64, blk * 512:(blk + 1) * 512], in_=ao[i2][:]),
                      reads=[ao[i2]])
        S.barrier()


def op_conv(S, nc, K, L):
    with ExitStack() as st:
        gin = [sb(nc, st, "cv_in%d" % i, [128, 542], F32) for i in range(3)]
        ck = [sb(nc, st, "cv_ck%d" % i, [128, 512], F32) for i in range(8)]
        sq = [sb(nc, st, "cv_sq%d" % i, [128, 512], F32) for i in range(2)]
        pm = ps(nc, st, "cv_pm", [128, 512])
        pq = ps(nc, st, "cv_pq", [128, 512])
        mean = sb(nc, st, "cv_mean", [128, 512], F32)
        rstd = sb(nc, st, "cv_rstd", [128, 512], F32)
        tmp = [sb(nc, st, "cv_tmp%d" % i, [128, 512], F32) for i in range(2)]
        ob = [sb(nc, st, "cv_ob%d" % i, [128, 512], BF16) for i in range(2)]
        n = 0
        for tt in range(T // 512):
            t0 = HALO + tt * 512 - 15
            for cc in range(8):
                g = gin[n % 3]
                n += 1
                S.dma("sp", lambda e, g=g, cc=cc, t0=t0: e.dma_start(out=g[:], in_=K.gluT[cc * 128:(cc + 1) * 128, t0:t0 + 542]), writes=[g])
                eng = "dve"
                acc = ck[cc]

                fns = [lambda e, g=g, cc=cc, acc=acc: e.tensor_scalar(out=acc[:], in0=g[:, 0:512], scalar1=K.wdw[:, L, cc, 0:1],
                                                                      scalar2=K.cvp[:, L, 0, cc:cc + 1], op0=ALU.mult, op1=ALU.add)]
                for k in range(1, 31):
                    fns.append(lambda e, g=g, cc=cc, acc=acc, k=k: e.scalar_tensor_tensor(
                        out=acc[:], in0=g[:, k:k + 512], scalar=K.wdw[:, L, cc, k:k + 1], in1=acc[:], op0=ALU.mult, op1=ALU.add))
                chain(S, eng, fns, reads=[g, K.wdw, K.cvp], writes=[acc])
                s2 = sq[cc % 2]
                S.op("act", lambda e, s2=s2, acc=acc: e.activation(out=s2[:], in_=acc[:], func=AF.Square), reads=[acc], writes=[s2])
                S.op("pe", lambda e, acc=acc, cc=cc: e.matmul(pm[:], K.ones_f[:], acc[:], start=(cc == 0), stop=(cc == 7)),
                     reads=[acc, K.ones_f], writes=[pm])
                S.op("pe", lambda e, s2=s2, cc=cc: e.matmul(pq[:], K.ones_f[:], s2[:], start=(cc == 0), stop=(cc == 7)),
                     reads=[s2, K.ones_f], writes=[pq])

            chain(S, "dve", [
                lambda e: e.tensor_scalar(out=mean[:], in0=pm[:], scalar1=1.0 / 1024, scalar2=None, op0=ALU.mult),
                lambda e: e.tensor_tensor(out=rstd[:], in0=mean[:], in1=mean[:], op=ALU.mult),
                lambda e: e.scalar_tensor_tensor(out=rstd[:], in0=pq[:], scalar=1.0 / 1024, in1=rstd[:], op0=ALU.mult, op1=ALU.subtract),
            ], reads=[pm, pq], writes=[mean, rstd])
            S.op("act", lambda e: e.activation(out=rstd[:], in_=rstd[:], func=AF.Sqrt, bias=K.epsc[:, 0:1]), reads=[rstd, K.epsc], writes=[rstd])
            S.op("dve", lambda e: e.reciprocal(out=rstd[:], in_=rstd[:]), reads=[rstd], writes=[rstd])
            for cc in range(8):
                tm = tmp[cc % 2]
                o = ob[cc % 2]
                acc = ck[cc]

                chain(S, "dve", [
                    lambda e, tm=tm, acc=acc: e.tensor_tensor(out=tm[:], in0=acc[:], in1=mean[:], op=ALU.subtract),
                    lambda e, tm=tm: e.tensor_tensor(out=tm[:], in0=tm[:], in1=rstd[:], op=ALU.mult),
                    lambda e, tm=tm, cc=cc: e.tensor_scalar(out=tm[:], in0=tm[:], scalar1=K.cvp[:, L, 1, cc:cc + 1],
                                                            scalar2=K.cvp[:, L, 2, cc:cc + 1], op0=ALU.mult, op1=ALU.add),
                ], reads=[acc, mean, rstd, K.cvp], writes=[tm])
                S.op("act", lambda e, tm=tm, o=o: e.activation(out=o[:], in_=tm[:], func=AF.Silu), reads=[tm], writes=[o])
                S.dma("pool", lambda e, o=o, cc=cc, tt=tt: e.dma_start(out=K.convT[cc * 128:(cc + 1) * 128, tt * 512:(tt + 1) * 512], in_=o[:]),
                      reads=[o])
        S.barrier()


def op_mix(S, nc, K, L):
    with ExitStack() as st:
        wa = sb(nc, st, "mx_wa", [128, 8, D], BF16)
        wc = sb(nc, st, "mx_wc", [128, 8, D], BF16)
        at = [sb(nc, st, "mx_at%d" % i, [128, 8, 512], BF16) for i in range(2)]
        cv = [sb(nc, st, "mx_cv%d" % i, [128, 8, 512], BF16) for i in range(2)]
        ga = [sb(nc, st, "mx_ga%d" % i, [128, 16, 512], BF16) for i in range(2)]
        gc = [sb(nc, st, "mx_gc%d" % i, [128, 16, 512], BF16) for i in range(2)]
        mt = [sb(nc, st, "mx_mt%d" % i, [128, 16, 512], BF16) for i in range(2)]
        t1 = [sb(nc, st, "mx_t1%d" % i, [128, 512], F32) for i in range(2)]
        t2 = [sb(nc, st, "mx_t2%d" % i, [128, 512], F32) for i in range(2)]
        pa = [ps(nc, st, "mx_pa%d" % i, [128, 512]) for i in range(2)]
        pb = [ps(nc, st, "mx_pb%d" % i, [128, 512]) for i in range(2)]
        S.dma("sp", lambda e: e.dma_start(out=wa[:], in_=fm(K.wb_ao)), writes=[wa])
        S.dma("sp", lambda e: e.dma_start(out=wc[:], in_=fm(K.wb_co)), writes=[wc])
        n = 0
        for tt in range(T // 512):
            i = tt % 2
            S.dma("sp", lambda e, i=i, tt=tt: e.dma_start(out=at[i][:], in_=fm(K.attT)[:, :, tt * 512:(tt + 1) * 512]), writes=[at[i]])
            S.dma("sp", lambda e, i=i, tt=tt: e.dma_start(out=cv[i][:], in_=fm(K.convT)[:, :, tt * 512:(tt + 1) * 512]), writes=[cv[i]])
            S.dma("sp", lambda e, i=i, tt=tt: e.dma_start(out=ga[i][:], in_=fm(K.gaT)[:, :, HALO + tt * 512:HALO + (tt + 1) * 512]), writes=[ga[i]])
            S.dma("sp", lambda e, i=i, tt=tt: e.dma_start(out=gc[i][:], in_=fm(K.gcT)[:, :, HALO + tt * 512:HALO + (tt + 1) * 512]), writes=[gc[i]])
            for oc in range(16):
                c = n % 2
                n += 1

                def f(e, c=c, i=i, oc=oc):
                    for kc in range(8):
                        e.matmul(pa[c][:], wa[:, kc, oc * 128:(oc + 1) * 128], at[i][:, kc, :], start=(kc == 0), stop=(kc == 7))
                    for kc in range(8):
                        ins = e.matmul(pb[c][:], wc[:, kc, oc * 128:(oc + 1) * 128], cv[i][:, kc, :], start=(kc == 0), stop=(kc == 7))
                    return ins
                S.op("pe", f, reads=[wa, wc, at[i], cv[i]], writes=[pa[c], pb[c]])
                S.op("dve", lambda e, c=c, i=i, oc=oc: e.tensor_tensor(out=t1[c][:], in0=pa[c][:], in1=ga[i][:, oc, :], op=ALU.mult),
                     reads=[pa[c], ga[i]], writes=[t1[c]])
                S.op("dve", lambda e, c=c, i=i, oc=oc: e.tensor_tensor(out=t2[c][:], in0=pb[c][:], in1=gc[i][:, oc, :], op=ALU.mult),
                     reads=[pb[c], gc[i]], writes=[t2[c]])
                S.op("pool", lambda e, c=c, i=i, oc=oc: e.tensor_tensor(out=mt[i][:, oc, :], in0=t1[c][:], in1=t2[c][:], op=ALU.add),
                     reads=[t1[c], t2[c]], writes=[mt[i]])
            S.dma("pool", lambda e, i=i, tt=tt: e.dma_start(out=fm(K.mT)[:, :, tt * 512:(tt + 1) * 512], in_=mt[i][:]), reads=[mt[i]])
        S.barrier()


def op_resid_linear(S, nc, K, inT, wbf, kchunks, xsrc, xsrc_off, xdst, tag):
    with ExitStack() as st:
        w = sb(nc, st, tag + "_w", [128, kchunks, D], BF16)
        it = [sb(nc, st, "%s_it%d" % (tag, i), [128, kchunks, 512], BF16) for i in range(2)]
        xt = [sb(nc, st, "%s_xt%d" % (tag, i), [128, D], F32) for i in range(2)]
        pp = [ps(nc, st, "%s_p%d" % (tag, i), [128, 512]) for i in range(4)]
        S.dma("sp", lambda e: e.dma_start(out=w[:], in_=fm(wbf)), writes=[w])
        n = 0
        for tt in range(T // 512):
            i = tt % 2
            S.dma("sp", lambda e, i=i, tt=tt: e.dma_start(out=it[i][:], in_=fm(inT)[:, :, tt * 512:(tt + 1) * 512]), writes=[it[i]])
            for s in range(4):
                x = xt[n % 2]
                n += 1
                t0 = tt * 512 + s * 128
                S.dma("sp", lambda e, x=x, t0=t0: e.dma_start(out=x[:], in_=xsrc[xsrc_off + t0:xsrc_off + t0 + 128, :]), writes=[x])
                for nb in range(4):
                    P = pp[nb]

                    def f(e, P=P, i=i, s=s, nb=nb):
                        for kc in range(kchunks):
                            ins = e.matmul(P[:], it[i][:, kc, s * 128:(s + 1) * 128], w[:, kc, nb * 512:(nb + 1) * 512],
                                           start=(kc == 0), stop=(kc == kchunks - 1))
                        return ins
                    S.op("pe", f, reads=[it[i], w], writes=[P])
                    S.op("dve", lambda e, P=P, x=x, nb=nb: e.tensor_tensor(out=x[:, nb * 512:(nb + 1) * 512], in0=P[:],
                                                                           in1=x[:, nb * 512:(nb + 1) * 512], op=ALU.add),
                         reads=[P, x], writes=[x])
                S.dma("pool", lambda e, x=x, t0=t0: e.dma_start(out=xdst[t0:t0 + 128, :], in_=x[:]), reads=[x])
        S.barrier()


def op_post(S, nc, K, L):
    with ExitStack() as st:
        xt = [sb(nc, st, "po_x%d" % i, [128, D], F32) for i in range(2)]
        junk = sb(nc, st, "po_junk", [128, D], F32)
        hf = [sb(nc, st, "po_hf%d" % i, [128, D], F32) for i in range(2)]
        hb = [sb(nc, st, "po_hb%d" % i, [128, D], BF16) for i in range(2)]
        ss = [sb(nc, st, "po_ss%d" % i, [128, 2], F32) for i in range(2)]
        g = sb(nc, st, "po_g", [128, D], F32)
        wr = sb(nc, st, "po_wr", [128, 16, 16], F32)
        hT = [sb(nc, st, "po_hT%d" % i, [128, 16, 128], F32) for i in range(2)]
        pt = [ps(nc, st, "po_pt%d" % i, [128, 512]) for i in range(2)]
        pl = [ps(nc, st, "po_pl%d" % i, [128, 16]) for i in range(2)]
        pa = ps(nc, st, "po_pa", [16, 128])
        sm = [sb(nc, st, "po_sm%d" % i, [128, 20], F32) for i in range(2)]
        af = [sb(nc, st, "po_af%d" % i, [128, 16], F32) for i in range(2)]
        affT = sb(nc, st, "po_affT", [16, T], F32)
        S.dma("sp", lambda e: e.dma_start(out=g[:], in_=K.gains[L, 1]), writes=[g])
        S.dma("sp", lambda e: e.dma_start(out=wr[:], in_=K.w_router[L].rearrange("(kc p) e -> p kc e", p=128)), writes=[wr])
        for tt in range(T // 128):
            i = tt % 2
            t0 = tt * 128
            S.dma("sp", lambda e, i=i, t0=t0: e.dma_start(out=xt[i][:], in_=K.x1[t0:t0 + 128, :]), writes=[xt[i]])
            S.op("act", lambda e, i=i: e.activation(out=junk[:], in_=xt[i][:], func=AF.Square), reads=[xt[i]], writes=[junk])
            S.op("dve", lambda e, i=i: e.tensor_reduce(out=ss[i][:, 0:1], in_=junk[:], axis=AX.X, op=ALU.add), reads=[junk], writes=[ss[i]])
            S.op("act", lambda e, i=i: e.activation(out=ss[i][:, 1:2], in_=ss[i][:, 0:1], func=AF.Sqrt, scale=1.0 / D, bias=K.epsc[:, 0:1]),
                 reads=[ss[i], K.epsc], writes=[ss[i]])
            S.op("dve", lambda e, i=i: e.reciprocal(out=ss[i][:, 1:2], in_=ss[i][:, 1:2]), reads=[ss[i]], writes=[ss[i]])
            S.op("dve", lambda e, i=i: e.scalar_tensor_tensor(out=hf[i][:], in0=xt[i][:], scalar=ss[i][:, 1:2], in1=g[:], op0=ALU.mult, op1=ALU.mult),
                 reads=[xt[i], ss[i], g], writes=[hf[i]])
            S.op("pool", lambda e, i=i: e.tensor_copy(out=hb[i][:], in_=hf[i][:]), reads=[hf[i]], writes=[hb[i]])
            S.dma("pool", lambda e, i=i, t0=t0: e.dma_start(out=K.h2own[t0:t0 + 128, :], in_=hb[i][:]), reads=[hb[i]])
            for q4 in range(4):
                P = pt[q4 % 2]

                def tr(e, P=P, i=i, q4=q4):
                    for k in range(4):
                        kc = q4 * 4 + k
                        ins = e.transpose(out=P[:, k * 128:(k + 1) * 128], in_=hf[i][:, kc * 128:(kc + 1) * 128], identity=K.identf[:])
                    return ins
                S.op("pe", tr, reads=[hf[i], K.identf], writes=[P])
                S.op("act", lambda e, P=P, i=i, q4=q4: e.activation(out=hT[i][:, q4 * 4:(q4 + 1) * 4, :],
                                                                    in_=P[:].rearrange("p (k t) -> p k t", k=4), func=AF.Copy),
                     reads=[P], writes=[hT[i]])

            def lg(e, i=i):
                for kc in range(16):
                    ins = e.matmul(pl[i][:], hT[i][:, kc, :], wr[:, kc, :], start=(kc == 0), stop=(kc == 15))
                return ins
            S.op("pe", lg, reads=[hT[i], wr], writes=[pl[i]])
            S.op("dve", lambda e, i=i: e.tensor_reduce(out=sm[i][:, 16:17], in_=pl[i][:], axis=AX.X, op=ALU.max), reads=[pl[i]], writes=[sm[i]])
            S.op("dve", lambda e, i=i: e.tensor_scalar(out=sm[i][:, 17:18], in0=sm[i][:, 16:17], scalar1=-1.0, scalar2=None, op0=ALU.mult),
                 reads=[sm[i]], writes=[sm[i]])
            S.op("act", lambda e, i=i: e.activation(out=sm[i][:, 0:16], in_=pl[i][:], func=AF.Exp, bias=sm[i][:, 17:18]),
                 reads=[pl[i], sm[i]], writes=[sm[i]])
            S.op("dve", lambda e, i=i: e.tensor_reduce(out=sm[i][:, 18:19], in_=sm[i][:, 0:16], axis=AX.X, op=ALU.add), reads=[sm[i]], writes=[sm[i]])
            S.op("dve", lambda e, i=i: e.reciprocal(out=sm[i][:, 19:20], in_=sm[i][:, 18:19]), reads=[sm[i]], writes=[sm[i]])
            S.op("dve", lambda e, i=i: e.tensor_scalar(out=af[i][:], in0=sm[i][:, 0:16], scalar1=sm[i][:, 19:20], scalar2=None, op0=ALU.mult),
                 reads=[sm[i]], writes=[af[i]])
            S.op("pe", lambda e, i=i: e.transpose(out=pa[:], in_=af[i][:], identity=K.identf[:]), reads=[af[i], K.identf], writes=[pa])
            S.op("act", lambda e, t0=t0: e.activation(out=affT[:, t0:t0 + 128], in_=pa[:], func=AF.Copy), reads=[pa], writes=[affT])
        S.dma("sp", lambda e: e.dma_start(out=K.affT_own.rearrange("(e c) t -> e (c t)", c=32), in_=affT[:]), reads=[affT])
        S.barrier()


def op_route(S, nc, K, L, st):
    A = sb(nc, st, "rt_A", [128, 4, 128], F32)
    cmpb = sb(nc, st, "rt_cmp", [128, 4, 128], F32)
    cum = sb(nc, st, "rt_cum", [128, 4, 128], F32)
    ones3 = sb(nc, st, "rt_ones", [128, 128], F32)
    sm = sb(nc, st, "rt_sm", [128, 8, 4], F32)
    pc = ps(nc, st, "rt_pc", [128, 4])
    rtab = sb(nc, st, "rt_rtab", [128, 4, 128, 6], BF16)
    r1 = sb(nc, st, "rt_r1", [128, 4, 128], F32)
    hi_f = sb(nc, st, "rt_hif", [128, 4, 128], F32)
    sel = [sb(nc, st, "rt_sel%d" % i, [128, CAP], BF16) for i in range(3)]
    pidx = [ps(nc, st, "rt_pidx%d" % i, [128, 16, 6]) for i in range(2)]
    res = sb(nc, st, "rt_res", [128, 16, 6], F32)
    idxf = sb(nc, st, "rt_idxf", [128, 16], F32)
    LO, HI, MID, CNT, GE, T1, T2, BASE = range(8)
    for j in range(4):
        S.dma("pool", lambda e, j=j: e.indirect_dma_start(
            out=A[:, j, :], out_offset=None, in_=K.affT_all,
            in_offset=bass.IndirectOffsetOnAxis(ap=K.ridx[:, j:j + 1], axis=0)), reads=[K.ridx], writes=[A])
    S.op("dve", lambda e: e.memset(ones3[:], 1.0), writes=[ones3])
    S.op("dve", lambda e: e.memset(sm[:, LO, :], 0.0), writes=[sm])
    S.op("dve", lambda e: e.memset(sm[:, HI, :], 1.0), writes=[sm])

    def count_ge(col):
        for j in range(4):
            S.op("dve", lambda e, j=j: e.tensor_scalar(out=cmpb[:, j, :], in0=A[:, j, :], scalar1=sm[:, col, j:j + 1], scalar2=None, op0=ALU.is_ge),
                 reads=[A, sm], writes=[cmpb])

    for it in range(36):
        S.op("dve", lambda e: e.tensor_tensor(out=sm[:, MID, :], in0=sm[:, LO, :], in1=sm[:, HI, :], op=ALU.add), reads=[sm], writes=[sm])
        S.op("dve", lambda e: e.tensor_scalar(out=sm[:, MID, :], in0=sm[:, MID, :], scalar1=0.5, scalar2=None, op0=ALU.mult), reads=[sm], writes=[sm])
        count_ge(MID)
        S.op("dve", lambda e: e.tensor_reduce(out=sm[:, CNT, :], in_=cmpb[:], axis=AX.X, op=ALU.add), reads=[cmpb], writes=[sm])
        S.op("pe", lambda e: e.matmul(pc[:], K.ones_f[:], sm[:, CNT, :], start=True, stop=True), reads=[K.ones_f, sm], writes=[pc])
        S.op("dve", lambda e: e.tensor_scalar(out=sm[:, GE, :], in0=pc[:], scalar1=float(CAP) - 0.5, scalar2=None, op0=ALU.is_ge), reads=[pc], writes=[sm])
        S.op("dve", lambda e: e.tensor_tensor(out=sm[:, T1, :], in0=sm[:, GE, :], in1=sm[:, MID, :], op=ALU.mult), reads=[sm], writes=[sm])
        S.op("dve", lambda e: e.tensor_tensor(out=sm[:, LO, :], in0=sm[:, LO, :], in1=sm[:, T1, :], op=ALU.max), reads=[sm], writes=[sm])
        S.op("dve", lambda e: e.tensor_scalar(out=sm[:, T2, :], in0=sm[:, GE, :], scalar1=2.0, scalar2=None, op0=ALU.mult), reads=[sm], writes=[sm])
        S.op("dve", lambda e: e.tensor_tensor(out=sm[:, T2, :], in0=sm[:, T2, :], in1=sm[:, MID, :], op=ALU.add), reads=[sm], writes=[sm])
        S.op("dve", lambda e: e.tensor_tensor(out=sm[:, HI, :], in0=sm[:, HI, :], in1=sm[:, T2, :], op=ALU.min), reads=[sm], writes=[sm])
    count_ge(LO)
    for j in range(4):
        S.op("dve", lambda e, j=j: e.tensor_tensor_scan(out=cum[:, j, :], data0=ones3[:], data1=cmpb[:, j, :], initial=0.0, op0=ALU.mult, op1=ALU.add),
             reads=[ones3, cmpb], writes=[cum])
    S.op("dve", lambda e: e.tensor_copy(out=sm[:, CNT, :], in_=cum[:, :, 127]), reads=[cum], writes=[sm])
    S.op("pe", lambda e: e.matmul(pc[:], K.tri[:], sm[:, CNT, :], start=True, stop=True), reads=[K.tri, sm], writes=[pc])
    S.op("dve", lambda e: e.tensor_copy(out=sm[:, BASE, :], in_=pc[:]), reads=[pc], writes=[sm])
    for j in range(4):
        S.op("dve", lambda e, j=j: e.tensor_scalar(out=cum[:, j, :], in0=cum[:, j, :], scalar1=sm[:, BASE, j:j + 1], scalar2=None, op0=ALU.add),
             reads=[cum, sm], writes=[cum])
        S.op("dve", lambda e, j=j: e.tensor_tensor(out=cum[:, j, :], in0=cum[:, j, :], in1=cmpb[:, j, :], op=ALU.mult), reads=[cum, cmpb], writes=[cum])
        S.op("dve", lambda e, j=j: e.tensor_scalar(out=cum[:, j, :], in0=cum[:, j, :], scalar1=-1.0, scalar2=None, op0=ALU.add), reads=[cum], writes=[cum])
    S.dma("pool", lambda e: e.dma_start(out=rtab[:].rearrange("p a b c -> p a (b c)"), in_=K.rtab_in.rearrange("p a b c -> p a (b c)")),
          writes=[rtab])
    S.op("dve", lambda e: e.tensor_copy(out=rtab[:, :, :, 3], in_=A[:]), reads=[A], writes=[rtab])
    S.op("dve", lambda e: e.tensor_copy(out=hi_f[:], in_=rtab[:, :, :, 3]), reads=[rtab], writes=[hi_f])
    S.op("dve", lambda e: e.tensor_tensor(out=r1[:], in0=A[:], in1=hi_f[:], op=ALU.subtract), reads=[A, hi_f], writes=[r1])
    S.op("dve", lambda e: e.tensor_copy(out=rtab[:, :, :, 4], in_=r1[:]), reads=[r1], writes=[rtab])
    S.op("dve", lambda e: e.tensor_copy(out=hi_f[:], in_=rtab[:, :, :, 4]), reads=[rtab], writes=[hi_f])
    S.op("dve", lambda e: e.tensor_tensor(out=r1[:], in0=r1[:], in1=hi_f[:], op=ALU.subtract), reads=[r1, hi_f], writes=[r1])
    S.op("dve", lambda e: e.tensor_copy(out=rtab[:, :, :, 5], in_=r1[:]), reads=[r1], writes=[rtab])
    n = 0
    for j in range(4):
        P = pidx[j % 2]
        for jj in range(128):
            sl = sel[n % 3]
            eng = "pool" if n % 3 == 2 else "dve"
            n += 1
            S.op(eng, lambda e, sl=sl, j=j, jj=jj: e.tensor_scalar(out=sl[:], in0=K.iota_s[:], scalar1=cum[:, j, jj:jj + 1], scalar2=None, op0=ALU.is_equal),
                 reads=[K.iota_s, cum], writes=[sl])

            def mm(e, sl=sl, j=j, jj=jj, P=P):
                for c in range(16):
                    ins = e.matmul(P[:, c, :], sl[:, c * 128:(c + 1) * 128], rtab[:, j, jj, :], start=(jj == 0 and c == 0),
                                   stop=(jj == 127 and c == 15), skip_group_check=True)
                return ins
            S.op("pe", mm, reads=[sl, rtab], writes=[P])
        S.op("act", lambda e, P=P: e.activation(out=res[:], in_=P[:], func=AF.Copy), reads=[P], writes=[res])
        S.op("dve", lambda e: e.tensor_tensor(out=idxf[:], in0=res[:, :, 0], in1=res[:, :, 2], op=ALU.add), reads=[res], writes=[idxf])
        S.op("dve", lambda e, j=j: e.tensor_copy(out=K.idx[:, j, :], in_=idxf[:]), reads=[idxf], writes=[K.idx])
        S.op("dve", lambda e: e.tensor_tensor(out=idxf[:], in0=res[:, :, 1], in1=res[:, :, 2], op=ALU.add), reads=[res], writes=[idxf])
        S.op("dve", lambda e, j=j: e.tensor_copy(out=K.idx_d[:, j, :], in_=idxf[:]), reads=[idxf], writes=[K.idx_d])
        S.op("dve", lambda e, j=j: e.tensor_tensor(out=K.gate[:, j, :], in0=res[:, :, 3], in1=res[:, :, 4], op=ALU.add), reads=[res], writes=[K.gate])
        S.op("dve", lambda e, j=j: e.tensor_tensor(out=K.gate[:, j, :], in0=K.gate[:, j, :], in1=res[:, :, 5], op=ALU.add), reads=[res, K.gate], writes=[K.gate])


def op_expert(S, nc, K, L, st):
    xg = [sb(nc, st, "ex_xg%d" % i, [128, D], BF16) for i in range(3)]
    xeT = sb(nc, st, "ex_xeT", [128, 16, 512], BF16)
    hidT = sb(nc, st, "ex_hidT", [128, 16, 512], BF16)
    w1b = [sb(nc, st, "ex_w1%d" % i, [128, 16, 256], BF16) for i in range(2)]
    w3b = [sb(nc, st, "ex_w3%d" % i, [128, 16, 256], BF16) for i in range(2)]
    w2f = sb(nc, st, "ex_w2f", [128, 16, D], BF16)
    sa = [sb(nc, st, "ex_sa%d" % i, [128, 512], F32) for i in range(2)]
    ysb = [sb(nc, st, "ex_y%d" % i, [128, D], F32) for i in range(2)]
    zt = sb(nc, st, "ex_z", [128, D], F32)
    pT = ps(nc, st, "ex_pT", [128, D], BF16)
    pa = [ps(nc, st, "ex_pa%d" % i, [128, 512]) for i in range(2)]
    pb = [ps(nc, st, "ex_pb%d" % i, [128, 512]) for i in range(2)]
    py = [ps(nc, st, "ex_py%d" % i, [128, 512]) for i in range(2)]
    delta = K.delta[L]
    S.op("dve", lambda e: e.memset(zt[:], 0.0), writes=[zt])
    for r in range(4 * T // 128):
        S.dma("sp", lambda e, r=r: e.dma_start(out=delta[r * 128:(r + 1) * 128, :], in_=zt[:]), reads=[zt], writes=[K.delta_buf])
    ng, nw, ny, nc2 = 0, 0, 0, 0
    for j in range(4):
        S.dma("sp", lambda e, j=j: e.dma_start(out=w2f[:], in_=fm(K.wb_2[j])), writes=[w2f])
        for rt in range(4):
            for s in range(4):
                c16 = rt * 4 + s
                x = xg[ng % 3]
                ng += 1
                S.dma("pool", lambda e, x=x, j=j, c16=c16: e.indirect_dma_start(
                    out=x[:], out_offset=None, in_=K.h2_all[L],
                    in_offset=bass.IndirectOffsetOnAxis(ap=K.idx[:, j, c16:c16 + 1], axis=0)), reads=[K.idx], writes=[x])

                def tr(e, x=x):
                    for kc in range(16):
                        ins = e.transpose(out=pT[:, kc * 128:(kc + 1) * 128], in_=x[:, kc * 128:(kc + 1) * 128], identity=K.identb[:])
                    return ins
                S.op("pe", tr, reads=[x, K.identb], writes=[pT])
                S.op("act", lambda e, s=s: e.activation(out=xeT[:, :, s * 128:(s + 1) * 128], in_=pT[:].rearrange("p (k t) -> p k t", k=16),
                                                        func=AF.Copy), reads=[pT], writes=[xeT])
            for fb in range(8):
                wi = nw % 2
                nw += 1
                S.dma("sp", lambda e, wi=wi, j=j, fb=fb: e.dma_start(out=w1b[wi][:], in_=fm(K.wb_1[j])[:, :, fb * 256:(fb + 1) * 256]), writes=[w1b[wi]])
                S.dma("sp", lambda e, wi=wi, j=j, fb=fb: e.dma_start(out=w3b[wi][:], in_=fm(K.wb_3[j])[:, :, fb * 256:(fb + 1) * 256]), writes=[w3b[wi]])
                for f2 in range(2):
                    fc = fb * 2 + f2
                    c = fc % 2

                    def mm(e, c=c, wi=wi, f2=f2):
                        for kc in range(16):
                            e.matmul(pa[c][:], w1b[wi][:, kc, f2 * 128:(f2 + 1) * 128], xeT[:, kc, :], start=(kc == 0), stop=(kc == 15))
                        for kc in range(16):
                            ins = e.matmul(pb[c][:], w3b[wi][:, kc, f2 * 128:(f2 + 1) * 128], xeT[:, kc, :], start=(kc == 0), stop=(kc == 15))
                        return ins
                    S.op("pe", mm, reads=[w1b[wi], w3b[wi], xeT], writes=[pa[c], pb[c]])
                    S.op("act", lambda e, c=c: e.activation(out=sa[c][:], in_=pa[c][:], func=AF.Silu), reads=[pa[c]], writes=[sa[c]])
                    S.op("dve", lambda e, c=c, fc=fc: e.tensor_tensor(out=hidT[:, fc, :], in0=pb[c][:], in1=sa[c][:], op=ALU.mult),
                         reads=[pb[c], sa[c]], writes=[hidT])
            for s in range(4):
                ys = ysb[ny % 2]
                ny += 1
                c16 = rt * 4 + s
                for nb in range(4):
                    c = nc2 % 2
                    nc2 += 1

                    def mm2(e, c=c, s=s, nb=nb):
                        for fc in range(16):
                            ins = e.matmul(py[c][:], hidT[:, fc, s * 128:(s + 1) * 128], w2f[:, fc, nb * 512:(nb + 1) * 512],
                                           start=(fc == 0), stop=(fc == 15))
                        return ins
                    S.op("pe", mm2, reads=[hidT, w2f], writes=[py[c]])
                    S.op("dve", lambda e, c=c, ys=ys, nb=nb, j=j, c16=c16: e.tensor_scalar(
                        out=ys[:, nb * 512:(nb + 1) * 512], in0=py[c][:], scalar1=K.gate[:, j, c16:c16 + 1], scalar2=None, op0=ALU.mult),
                        reads=[py[c], K.gate], writes=[ys])
                S.dma("pool", lambda e, ys=ys, j=j, c16=c16: e.indirect_dma_start(
                    out=delta, out_offset=bass.IndirectOffsetOnAxis(ap=K.idx_d[:, j, c16:c16 + 1], axis=0), in_=ys[:], in_offset=None,
                    compute_op=ALU.add), reads=[ys, K.idx_d], writes=[K.delta_buf])


def op_ple(S, nc, K, L, xdst, xdst_off):
    with ExitStack() as st:
        wpg = sb(nc, st, "pl_wpg", [128, 16, D], BF16)
        wpe = sb(nc, st, "pl_wpe", [128, 2, D], BF16)
        g = sb(nc, st, "pl_g", [128, D], F32)
        xt = [sb(nc, st, "pl_x%d" % i, [128, D], F32) for i in range(2)]
        dl = [sb(nc, st, "pl_d%d" % i, [128, D], F32) for i in range(2)]
        junk = sb(nc, st, "pl_junk", [128, D], F32)
        xn = sb(nc, st, "pl_xn", [128, D], BF16)
        xrT = sb(nc, st, "pl_xrT", [128, 16, 128], BF16)
        pTt = [sb(nc, st, "pl_pT%d" % i, [128, 2, 128], BF16) for i in range(2)]
        pe_sb = sb(nc, st, "pl_pe", [128, D], F32)
        sg = sb(nc, st, "pl_sg", [128, D], F32)
        ss = [sb(nc, st, "pl_ss%d" % i, [128, 4], F32) for i in range(2)]
        pT = ps(nc, st, "pl_pTp", [128, D], BF16)
        pp = [ps(nc, st, "pl_pp%d" % i, [128, 512]) for i in range(2)]
        pg = [ps(nc, st, "pl_pg%d" % i, [128, 512]) for i in range(2)]
        S.dma("sp", lambda e: e.dma_start(out=wpg[:], in_=fm(K.wb_pg)), writes=[wpg])
        S.dma("sp", lambda e: e.dma_start(out=wpe[:], in_=fm(K.wb_pe)), writes=[wpe])
        S.dma("sp", lambda e: e.dma_start(out=g[:], in_=K.gains[L, 2]), writes=[g])
        n = 0
        for tt in range(T // 128):
            i = tt % 2
            t0 = tt * 128
            x, d = xt[i], dl[i]
            S.dma("sp", lambda e, x=x, t0=t0: e.dma_start(out=x[:], in_=K.x1[t0:t0 + 128, :]), writes=[x])
            S.dma("sp", lambda e, d=d, t0=t0: e.dma_start(out=d[:], in_=K.delta_own[L][t0:t0 + 128, :]), writes=[d])
            S.dma("pool", lambda e, i=i, t0=t0: e.dma_start(out=pTt[i][:], in_=K.pT_in[L].rearrange("(kc p) t -> p kc t", p=128)[:, :, t0:t0 + 128]),
                  writes=[pTt[i]])
            S.op("dve", lambda e, x=x, d=d: e.tensor_tensor(out=x[:], in0=x[:], in1=d[:], op=ALU.add), reads=[x, d], writes=[x])
            S.op("act", lambda e, x=x: e.activation(out=junk[:], in_=x[:], func=AF.Square), reads=[x], writes=[junk])
            S.op("dve", lambda e, i=i: e.tensor_reduce(out=ss[i][:, 0:1], in_=junk[:], axis=AX.X, op=ALU.add), reads=[junk], writes=[ss[i]])
            S.op("act", lambda e, i=i: e.activation(out=ss[i][:, 1:2], in_=ss[i][:, 0:1], func=AF.Sqrt, scale=1.0 / D, bias=K.epsc[:, 0:1]),
                 reads=[ss[i], K.epsc], writes=[ss[i]])
            S.op("dve", lambda e, i=i: e.reciprocal(out=ss[i][:, 1:2], in_=ss[i][:, 1:2]), reads=[ss[i]], writes=[ss[i]])
            S.op("dve", lambda e, i=i, x=x: e.tensor_scalar(out=xn[:], in0=x[:], scalar1=ss[i][:, 1:2], scalar2=None, op0=ALU.mult),
                 reads=[x, ss[i]], writes=[xn])

            def tr(e):
                for kc in range(16):
                    ins = e.transpose(out=pT[:, kc * 128:(kc + 1) * 128], in_=xn[:, kc * 128:(kc + 1) * 128], identity=K.identb[:])
                return ins
            S.op("pe", tr, reads=[xn, K.identb], writes=[pT])
            S.op("act", lambda e: e.activation(out=xrT[:], in_=pT[:].rearrange("p (k t) -> p k t", k=16), func=AF.Copy), reads=[pT], writes=[xrT])
            for nb in range(4):
                c = n % 2
                n += 1

                def mm(e, c=c, nb=nb, i=i):
                    for kc in range(2):
                        e.matmul(pp[c][:], pTt[i][:, kc, :], wpe[:, kc, nb * 512:(nb + 1) * 512], start=(kc == 0), stop=(kc == 1))
                    for kc in range(16):
                        ins = e.matmul(pg[c][:], xrT[:, kc, :], wpg[:, kc, nb * 512:(nb + 1) * 512], start=(kc == 0), stop=(kc == 15))
                    return ins
                S.op("pe", mm, reads=[pTt[i], wpe, xrT, wpg], writes=[pp[c], pg[c]])
                S.op("act", lambda e, c=c, nb=nb: e.activation(out=pe_sb[:, nb * 512:(nb + 1) * 512], in_=pp[c][:], func=AF.Copy),
                     reads=[pp[c]], writes=[pe_sb])
                S.op("act", lambda e, c=c, nb=nb: e.activation(out=sg[:, nb * 512:(nb + 1) * 512], in_=pg[c][:], func=AF.Sigmoid),
                     reads=[pg[c]], writes=[sg])
            S.op("act", lambda e: e.activation(out=junk[:], in_=pe_sb[:], func=AF.Square), reads=[pe_sb], writes=[junk])
            S.op("dve", lambda e, i=i: e.tensor_reduce(out=ss[i][:, 2:3], in_=junk[:], axis=AX.X, op=ALU.add), reads=[junk], writes=[ss[i]])
            S.op("act", lambda e, i=i: e.activation(out=ss[i][:, 3:4], in_=ss[i][:, 2:3], func=AF.Sqrt, scale=1.0 / D, bias=K.epsc[:, 0:1]),
                 reads=[ss[i], K.epsc], writes=[ss[i]])
            S.op("dve", lambda e, i=i: e.reciprocal(out=ss[i][:, 3:4], in_=ss[i][:, 3:4]), reads=[ss[i]], writes=[ss[i]])
            S.op("dve", lambda e, i=i: e.scalar_tensor_tensor(out=pe_sb[:], in0=pe_sb[:], scalar=ss[i][:, 3:4], in1=g[:], op0=ALU.mult, op1=ALU.mult),
                 reads=[pe_sb, ss[i], g], writes=[pe_sb])
            S.op("dve", lambda e: e.tensor_tensor(out=pe_sb[:], in0=pe_sb[:], in1=sg[:], op=ALU.mult), reads=[pe_sb, sg], writes=[pe_sb])
            S.op("dve", lambda e, x=x: e.tensor_tensor(out=x[:], in0=x[:], in1=pe_sb[:], op=ALU.add), reads=[x, pe_sb], writes=[x])
            S.dma("pool", lambda e, x=x, t0=t0: e.dma_start(out=xdst[xdst_off + t0:xdst_off + t0 + 128, :], in_=x[:]), reads=[x])
        S.barrier()


def op_halo(S, nc, K, xext):
    with ExitStack() as st:
        t = [sb(nc, st, "hl_t%d" % i, [128, D], F32) for i in range(2)]
        for k in range(4):
            src0 = (HALO + T - 256 + k * 128) if k < 2 else (HALO + (k - 2) * 128)
            b = t[k % 2]
            S.dma("sp", lambda e, b=b, src0=src0: e.dma_start(out=b[:], in_=xext[src0:src0 + 128, :]), writes=[b])
            S.dma("sp", lambda e, b=b, k=k: e.dma_start(out=K.hsend[k * 128:(k + 1) * 128, :], in_=b[:]), reads=[b])
        S.barrier()
        for k in range(4):
            S.coll(lambda e, k=k: e.collective_compute("AllGather", ALU.bypass, replica_groups=K.groups,
                                                       ins=[K.hsend[k * 128:(k + 1) * 128, :]], outs=[K.hall[k * 512:(k + 1) * 512, :]]))
        S.barrier()
        for k in range(4):
            b = t[k % 2]
            S.dma("pool", lambda e, b=b, k=k: e.indirect_dma_start(
                out=b[:], out_offset=None, in_=K.hall, in_offset=bass.IndirectOffsetOnAxis(ap=K.hidx[:, k:k + 1], axis=0)),
                reads=[K.hidx], writes=[b])
            S.op("dve", lambda e, b=b, k=k: e.tensor_scalar(out=b[:], in0=b[:], scalar1=K.hmask[:, k // 2:k // 2 + 1], scalar2=None, op0=ALU.mult),
                 reads=[b, K.hmask], writes=[b])
            dst0 = k * 128 if k < 2 else HALO + T + (k - 2) * 128
            S.dma("sp", lambda e, b=b, dst0=dst0: e.dma_start(out=xext[dst0:dst0 + 128, :], in_=b[:]), reads=[b])
        S.barrier()


def build_program(dbg=(), nlayers=NL, nli=NL):
    nc = bass.Bass("TRN2", target_bir_lowering=False)
    K = Ctx()
    _CAST_BUF.w, _CAST_BUF.r = None, []

    def din(name, shape, dt=F32):
        return nc.dram_tensor(name, list(shape), dt, kind="ExternalInput").ap()

    def dscr(name, shape, dt):
        kind = "ExternalOutput" if name in dbg else "Internal"
        return nc.dram_tensor(name, list(shape), dt, kind=kind).ap()

    xe0 = din("xe0", [TE, D])
    w_in = din("w_in", [nli, D, NIN])
    w_ao = din("w_att_o", [nli, 1024, D])
    w_co = din("w_conv_o", [nli, 1024, D])
    w_out = din("w_out", [nli, D, D])
    w_pg = din("w_pg", [nli, D, D])
    w_pe = din("w_pe", [nli, 256, D])
    K.w_router = din("w_router", [nli, D, 16])
    w1s = din("w1s", [nli, 4, D, D])
    w3s = din("w3s", [nli, 4, D, D])
    w2s = din("w2s", [nli, 4, D, D])
    K.pT_in = din("pT_in", [nli, 256, T])
    K.gains = din("gains", [nli, 3, 128, D])
    qkg = din("qkg_in", [128, nli, 2])
    wdw = din("wdw_in", [128, nli, 8, 31])
    cvp = din("cvp_in", [128, nli, 3, 8])
    K.att_tab = din("att_tab", [nli, 16, 128, 3, ATT_COLS])
    identb_d = din("identb_in", [128, 128])
    blk64_d = din("blk64_in", [128, 128])
    tri_d = din("tri_in", [128, 128])
    iota_d = din("iota_in", [128, CAP])
    K.rtab_in = din("rtab_in", [128, 4, 128, 6])
    ridx_d = din("ridx_in", [128, 4], I32)
    hidx_d = din("hidx_in", [128, 4], I32)
    hmask_d = din("hmask_in", [128, 2])
    y = nc.dram_tensor("y", [T, D], F32, kind="ExternalOutput").ap()

    K.wb_in = dscr("wb_in", [D, NIN], BF16)
    K.wb_ao = dscr("wb_ao", [1024, D], BF16)
    K.wb_co = dscr("wb_co", [1024, D], BF16)
    K.wb_out = dscr("wb_out", [D, D], BF16)
    K.wb_pg = dscr("wb_pg", [D, D], BF16)
    K.wb_pe = dscr("wb_pe", [256, D], BF16)
    K.wb_1 = dscr("wb_1", [4, D, D], BF16)
    K.wb_3 = dscr("wb_3", [4, D, D], BF16)
    K.wb_2 = dscr("wb_2", [4, D, D], BF16)
    K.hT = dscr("hT", [D, TE], BF16)
    K.qT = dscr("qT", [1024, TE], BF16)
    K.kT = dscr("kT", [1024, TE], BF16)
    K.v = dscr("v", [TE, 1024], BF16)
    K.gluT = dscr("gluT", [1024, TE], F32)
    K.gaT = dscr("gaT", [D, TE], BF16)
    K.gcT = dscr("gcT", [D, TE], BF16)
    K.attT = dscr("attT", [1024, T], BF16)
    K.convT = dscr("convT", [1024, T], BF16)
    K.mT = dscr("mT", [D, T], BF16)
    x1s = [dscr("x1_%d" % l, [T, D], F32) for l in range(NL)]
    affs = [dscr("affT_own%d" % l, [512, 128], F32) for l in range(NL)]
    xext1 = dscr("xext1", [TE, D], F32)
    K.h2own = dscr("h2own", [T, D], BF16)
    K.affT_all = dscr("affT_all", [2048, 128], F32)
    K.h2_all = [dscr("h2_all%d" % l, [4 * T, D], BF16) for l in range(NL)]
    K.delta = [dscr("delta%d" % l, [4 * T, D], F32) for l in range(NL)]
    K.delta_own = [dscr("delta_own%d" % l, [T, D], F32) for l in range(NL)]
    K.hsend = dscr("hsend", [512, D], F32)
    K.hall = dscr("hall", [2048, D], F32)
    K.groups = [[0, 1, 2, 3], [4, 5, 6, 7]]
    K.delta_buf = Buf(None)

    with ExitStack() as st:
        S = Sched(nc)
        K.identb = sb(nc, st, "identb", [128, 128], BF16)
        K.identf = sb(nc, st, "identf", [128, 128], F32)
        K.blk64 = sb(nc, st, "blk64", [128, 128], F32)
        K.tri = sb(nc, st, "tri", [128, 128], F32)
        K.ones_f = sb(nc, st, "ones_f", [128, 128], F32)
        K.ones_b = sb(nc, st, "ones_b", [128, 128], BF16)
        K.qkg_t = sb(nc, st, "qkg_t", [128, nli, 2], F32)
        K.qg = sb(nc, st, "qg", [128, 1], F32)
        K.kg = sb(nc, st, "kg", [128, 1], F32)
        K.wdw = sb(nc, st, "wdw", [128, nli, 8, 31], F32)
        K.cvp = sb(nc, st, "cvp", [128, nli, 3, 8], F32)
        K.ridx = sb(nc, st, "ridx", [128, 4], I32)
        K.hidx = sb(nc, st, "hidx", [128, 4], I32)
        K.hmask = sb(nc, st, "hmask", [128, 2], F32)
        K.epsc = sb(nc, st, "epsc", [128, 1], F32)
        S.dma("pool", lambda e: e.dma_start(out=K.identb[:], in_=identb_d), writes=[K.identb])
        S.dma("sp", lambda e: e.dma_start(out=K.identf[:], in_=identb_d), writes=[K.identf])
        S.dma("sp", lambda e: e.dma_start(out=K.blk64[:], in_=blk64_d), writes=[K.blk64])
        S.dma("sp", lambda e: e.dma_start(out=K.tri[:], in_=tri_d), writes=[K.tri])
        S.dma("sp", lambda e: e.dma_start(out=K.qkg_t[:], in_=qkg), writes=[K.qkg_t])
        S.dma("sp", lambda e: e.dma_start(out=K.wdw[:], in_=wdw), writes=[K.wdw])
        S.dma("sp", lambda e: e.dma_start(out=K.cvp[:], in_=cvp), writes=[K.cvp])
        S.dma("sp", lambda e: e.dma_start(out=K.ridx[:], in_=ridx_d), writes=[K.ridx])
        S.dma("sp", lambda e: e.dma_start(out=K.hidx[:], in_=hidx_d), writes=[K.hidx])
        S.dma("sp", lambda e: e.dma_start(out=K.hmask[:], in_=hmask_d), writes=[K.hmask])
        S.op("dve", lambda e: e.memset(K.epsc[:], EPS), writes=[K.epsc])
        S.op("dve", lambda e: e.memset(K.ones_f[:], 1.0), writes=[K.ones_f])
        S.op("dve", lambda e: e.memset(K.ones_b[:], 1.0), writes=[K.ones_b])
        S.barrier()

        for L in range(nlayers):
            last = L == nlayers - 1
            xin = xe0 if L == 0 else xext1
            K.x1 = x1s[L]
            K.affT_own = affs[L]
            S.op("dve", lambda e, L=L: e.tensor_scalar(out=K.qg[:], in0=K.qkg_t[:, L, 0:1], scalar1=0.125, scalar2=None, op0=ALU.mult),
                 reads=[K.qkg_t], writes=[K.qg])
            S.op("dve", lambda e, L=L: e.tensor_copy(out=K.kg[:], in_=K.qkg_t[:, L, 1:2]), reads=[K.qkg_t], writes=[K.kg])
            op_cast(S, w_in[L], K.wb_in, D, NIN)
            op_cast(S, w_ao[L], K.wb_ao, 1024, D)
            op_cast(S, w_co[L], K.wb_co, 1024, D)
            op_cast(S, w_out[L], K.wb_out, D, D)
            op_cast(S, w_pg[L], K.wb_pg, D, D)
            op_cast(S, w_pe[L], K.wb_pe, 256, D)
            for j in range(4):
                op_cast(S, w1s[L, j], K.wb_1[j], D, D)
                op_cast(S, w3s[L, j], K.wb_3[j], D, D)
                op_cast(S, w2s[L, j], K.wb_2[j], D, D)
            S.barrier()
            op_norm_T(S, nc, K, xin, TE, K.gains[L, 0], K.hT, "n1")
            op_inproj(S, nc, K, L)
            op_attention(S, nc, K, L)
            op_conv(S, nc, K, L)
            op_mix(S, nc, K, L)
            op_resid_linear(S, nc, K, K.mT, K.wb_out, 16, xin, HALO, K.x1, "ro")
            op_post(S, nc, K, L)
            S.coll(lambda e: e.collective_compute("AllGather", ALU.bypass, replica_groups=K.groups, ins=[K.affT_own], outs=[K.affT_all]))
            for k in range(16):
                S.coll(lambda e, L=L, k=k: e.collective_compute("AllGather", ALU.bypass, replica_groups=K.groups,
                                                                ins=[K.h2own[k * 256:(k + 1) * 256, :]], outs=[K.h2_all[L][k * 1024:(k + 1) * 1024, :]]))
            S.barrier()
            with ExitStack() as st2:
                K.idx = sb(nc, st2, "idx", [128, 4, 16], I32)
                K.idx_d = sb(nc, st2, "idx_d", [128, 4, 16], I32)
                K.gate = sb(nc, st2, "gate", [128, 4, 16], F32)
                K.iota_s = sb(nc, st2, "iota_s", [128, CAP], F32)
                S.dma("sp", lambda e: e.dma_start(out=K.iota_s[:], in_=iota_d), writes=[K.iota_s])
                with ExitStack() as st3:
                    op_route(S, nc, K, L, st3)
                    S.barrier()
                with ExitStack() as st3:
                    op_expert(S, nc, K, L, st3)
                    S.barrier()
            for k in range(32):
                S.coll(lambda e, L=L, k=k: e.collective_compute("ReduceScatter", ALU.add, replica_groups=K.groups,
                                                                ins=[K.delta[L][k * 512:(k + 1) * 512, :]], outs=[K.delta_own[L][k * 128:(k + 1) * 128, :]]))
            S.barrier()
            if last:
                op_ple(S, nc, K, L, y, 0)
            else:
                op_ple(S, nc, K, L, xext1, HALO)
                op_halo(S, nc, K, xext1)
        S.barrier()
        S.close()
    return nc


def _att_tables(rpb):
    Lr = rpb.shape[0]
    tabs = np.full((3, Lr, 16, 128, ATT_COLS), NEG, np.float32)
    kr = np.arange(128) // 64
    kc = np.arange(128) % 64
    for slot in range(3):
        o = 0
        for ci, (a, b) in enumerate(ATT_RNG):
            rel = 2 * ci - 4 + kr
            for i in range(a, b + 1):
                if (slot == 0 and i < 4) or (slot == 2 and i >= 4):
                    ok_r = (rel >= 0) & (rel <= 7)
                else:
                    ok_r = (rel >= i - 4) & (rel <= i + 3)
                dr = rel - i
                for qc in range(64):
                    cs = min(max(qc - 8, 0), 48)
                    ok = ok_r & (kc >= cs) & (kc < cs + 16)
                    idx = np.nonzero(ok)[0]
                    if idx.size == 0:
                        continue
                    col = o + (i - a) * 64 + qc
                    tabs[slot][:, :, idx, col] = rpb[:, :, dr[idx] + 7, kc[idx] - qc + 15]
            o += (b - a + 1) * 64
    return tabs


def _shared(inp):
    g = np.stack([inp["norm1_g"], inp["norm2_g"], inp["pe_norm_g"]], axis=1)
    gains = np.ascontiguousarray(np.broadcast_to(g[:, :, None, :], (NL, 3, 128, D))).astype(np.float32)
    qkg = np.zeros((128, NL, 2), np.float32)
    for l in range(NL):
        qkg[:, l, 0] = np.tile(inp["q_norm_g"][l], 2)
        qkg[:, l, 1] = np.tile(inp["k_norm_g"][l], 2)
    wdw = np.ascontiguousarray(inp["w_dw"].reshape(NL, 31, 8, 128).transpose(3, 0, 2, 1)).astype(np.float32)
    cvp = np.stack([inp["b_dw"], inp["conv_ln_g"], inp["conv_ln_b"]], axis=1)
    cvp = np.ascontiguousarray(cvp.reshape(NL, 3, 8, 128).transpose(3, 0, 1, 2)).astype(np.float32)
    blk64 = np.zeros((128, 128), np.float32)
    blk64[:64, :64] = 1.0
    blk64[64:, 64:] = 1.0
    tri = np.triu(np.ones((128, 128), np.float32), 1)
    iota = np.ascontiguousarray(np.broadcast_to(np.arange(CAP, dtype=np.float32)[None, :], (128, CAP)))
    rtab = np.zeros((128, 4, 128, 6), np.float32)
    pa = np.arange(128)
    rr, pp = pa // 32, pa % 32
    g_h2 = (pp // 2) * 1024 + rr * 256 + (pp % 2) * 128
    g_dl = pp * 512 + rr * 128
    rtab[:, :, :, 0] = g_h2.astype(np.float32)[:, None, None]
    rtab[:, :, :, 1] = g_dl.astype(np.float32)[:, None, None]
    rtab[:, :, :, 2] = np.arange(128, dtype=np.float32)[None, None, :]
    common = {
        "w_in": inp["w_in"], "w_att_o": inp["w_att_o"], "w_conv_o": inp["w_conv_o"], "w_out": inp["w_out"],
        "w_pg": inp["w_pg"], "w_pe": inp["w_pe"], "w_router": inp["w_router"],
        "gains": gains, "qkg_in": qkg, "wdw_in": wdw, "cvp_in": cvp,
        "identb_in": np.eye(128, dtype=np.float32), "blk64_in": blk64, "tri_in": tri, "iota_in": iota, "rtab_in": rtab,
    }
    return {"common": common, "tabs": _att_tables(np.asarray(inp["rpb"], np.float32))}


def _prep_core(c, inp, shared):
    b, q = c // 4, c % 4
    x = inp["x"]
    xe = np.zeros((TE, D), np.float32)
    lo = q * T - HALO
    hi = (q + 1) * T + HALO
    s0, s1 = max(lo, 0), min(hi, 4 * T)
    xe[s0 - lo:s1 - lo] = x[b, s0:s1]
    tabs = shared["tabs"]
    sel = [0 if q == 0 else 1, 1, 2 if q == 3 else 1]
    att_tab = np.stack([tabs[s] for s in sel], axis=3)
    m = dict(shared["common"])
    m["xe0"] = xe
    m["att_tab"] = np.ascontiguousarray(att_tab)
    m["w1s"] = np.ascontiguousarray(inp["w1"][:, 4 * q:4 * q + 4])
    m["w3s"] = np.ascontiguousarray(inp["w3"][:, 4 * q:4 * q + 4])
    m["w2s"] = np.ascontiguousarray(inp["w2"][:, 4 * q:4 * q + 4])
    m["pT_in"] = np.ascontiguousarray(inp["p"][:, b, q * T:(q + 1) * T, :].transpose(0, 2, 1))
    pidx = np.arange(128)
    r, pp = pidx // 32, pidx % 32
    ridx = np.zeros((128, 4), np.int32)
    for j in range(4):
        ridx[:, j] = (r * 16 + 4 * q + j) * 32 + pp
    hidx = np.zeros((128, 4), np.int32)
    hmask = np.zeros((128, 2), np.float32)
    if q > 0:
        hmask[:, 0] = 1.0
        for k in range(2):
            hidx[:, k] = k * 512 + (q - 1) * 128 + pidx
    if q < 3:
        hmask[:, 1] = 1.0
        for k in range(2):
            hidx[:, 2 + k] = (2 + k) * 512 + (q + 1) * 128 + pidx
    m["ridx_in"] = ridx
    m["hidx_in"] = hidx
    m["hmask_in"] = hmask
    return m


_LEAD = ("w_in", "w_att_o", "w_conv_o", "w_out", "w_pg", "w_pe", "w_router", "w1s", "w3s", "w2s", "pT_in", "gains", "att_tab")
_SECOND = ("qkg_in", "wdw_in", "cvp_in")
FUSED = True


def _layer_slice(m, L):
    o = dict(m)
    for k in _LEAD:
        o[k] = np.ascontiguousarray(m[k][L:L + 1])
    for k in _SECOND:
        o[k] = np.ascontiguousarray(m[k][:, L:L + 1])
    return o


def _gather_out(res):
    out = np.zeros((2, 4 * T, D), np.float32)
    for c in range(NCORES):
        out[c // 4, (c % 4) * T:(c % 4 + 1) * T] = res.results[c]["y"]
    return out


def kernel(**inputs):
    inp = {k: np.asarray(v) for k, v in inputs.items()}
    shared = _shared(inp)
    if FUSED:
        nc = build_program()
        in_maps = [_prep_core(c, inp, shared) for c in range(NCORES)]
        return _gather_out(run_bass_kernel_spmd(nc, in_maps, core_ids=list(range(NCORES))))
    x = inp["x"]
    for L in range(NL):
        cur = dict(inp)
        cur["x"] = x
        nc = build_program(nlayers=1, nli=1)
        in_maps = [_layer_slice(_prep_core(c, cur, shared), L) for c in range(NCORES)]
        x = _gather_out(run_bass_kernel_spmd(nc, in_maps, core_ids=list(range(NCORES))))
    return x
```

```python
from contextlib import ExitStack

import numpy as np
import concourse.bass as bass
import concourse.mybir as mybir
from concourse.bass_utils import run_bass_kernel_spmd

F32 = mybir.dt.float32
BF16 = mybir.dt.bfloat16
I32 = mybir.dt.int32
U32 = mybir.dt.uint32
AF = mybir.ActivationFunctionType
ALU = mybir.AluOpType
AX = mybir.AxisListType

D = 2048
T = 4096
HALO = 256
TE = T + 2 * HALO
NIN = 9216
EPS = 1e-6
NL = 2
NCORES = 8
CAP = 2048
NEG = -30000.0

ATT_RNG = [(0, 1), (0, 3), (0, 7), (0, 7), (0, 7), (0, 7), (5, 7), (7, 7)]
ATT_ORDER = [2, 3, 4, 5, 0, 1, 6, 7]
ATT_COLS = sum((b - a + 1) * 64 for a, b in ATT_RNG)


class Buf:
    __slots__ = ("t", "w", "r")

    def __init__(self, t):
        self.t = t
        self.w = None
        self.r = []

    def __getitem__(self, k):
        return self.t[k]


class Sched:
    def __init__(self, nc, n_dma_sems=8):
        self.nc = nc
        self.eng = {"pe": nc.tensor, "act": nc.scalar, "dve": nc.vector, "pool": nc.gpsimd, "sp": nc.sync}
        self.sem, self.cnt, self._ctx = {}, {}, []
        for e in self.eng:
            cm = nc.semaphore("s_" + e)
            self.sem[e] = cm.__enter__()
            self._ctx.append(cm)
            self.cnt[e] = 0
        self.dsem, self.dcnt, self.dnext, self.dtok = {}, {}, {}, {}
        for q in ("sp", "pool", "act"):
            self.dsem[q] = []
            for i in range(n_dma_sems):
                cm = nc.semaphore("d_%s%d" % (q, i))
                self.dsem[q].append(cm.__enter__())
                self._ctx.append(cm)
            self.dcnt[q] = [0] * n_dma_sems
            self.dtok[q] = [None] * n_dma_sems
            self.dnext[q] = 0
        self.seen = {e: {} for e in self.eng}

    def close(self):
        for cm in reversed(self._ctx):
            cm.__exit__(None, None, None)

    def _wait(self, e, tok):
        if tok is None:
            return
        s, v = tok
        k = id(s)
        if self.seen[e].get(k, 0) >= v:
            return
        self.seen[e][k] = v
        self.eng[e].wait_ge(s, v)

    def _deps(self, e, reads, writes):
        for b in reads:
            self._wait(e, b.w)
        for b in writes:
            self._wait(e, b.w)
            for t in b.r:
                self._wait(e, t)

    def _commit(self, tok, reads, writes):
        for b in reads:
            b.r.append(tok)
            if len(b.r) > 32:
                b.r = b.r[-32:]
        for b in writes:
            b.w = tok
            b.r = []

    def op(self, e, fn, reads=(), writes=()):
        self._deps(e, reads, writes)
        inst = fn(self.eng[e])
        self.cnt[e] += 1
        inst.then_inc(self.sem[e], 1)
        tok = (self.sem[e], self.cnt[e])
        self._commit(tok, reads, writes)
        return tok

    def dma(self, q, fn, reads=(), writes=()):
        i = self.dnext[q]
        self.dnext[q] = (i + 1) % len(self.dsem[q])
        self._wait(q, self.dtok[q][i])
        self._deps(q, reads, writes)
        inst = fn(self.eng[q])
        self.dcnt[q][i] += 16
        inst.then_inc(self.dsem[q][i], 16)
        tok = (self.dsem[q][i], self.dcnt[q][i])
        self.dtok[q][i] = tok
        self._commit(tok, reads, writes)
        return tok

    def coll(self, fn, reads=(), writes=()):
        if "cc" not in self.sem:
            cm = self.nc.semaphore("s_cc")
            self.sem["cc"] = cm.__enter__()
            self._ctx.append(cm)
            self.cnt["cc"] = 0
        self._deps("pool", reads, writes)
        inst = fn(self.eng["pool"])
        self.cnt["cc"] += 1
        inst.then_inc(self.sem["cc"])
        tok = (self.sem["cc"], self.cnt["cc"])
        self._commit(tok, reads, writes)
        return tok

    def barrier(self, engines=None):
        toks = []
        if self.cnt.get("cc"):
            toks.append((self.sem["cc"], self.cnt["cc"]))
        for e in self.eng:
            if self.cnt[e]:
                toks.append((self.sem[e], self.cnt[e]))
        for q in self.dsem:
            for t in self.dtok[q]:
                if t is not None:
                    toks.append(t)
        for e in (engines or self.eng):
            for t in toks:
                self._wait(e, t)


class Ctx:
    pass


def chain(S, e, fns, reads, writes):
    for f in fns:
        S.op(e, f, reads=reads, writes=writes)


_UID = [0]


def _uname(name):
    _UID[0] += 1
    return "%s_u%d" % (name, _UID[0])


def sb(nc, st, name, shape, dt):
    return Buf(st.enter_context(nc.sbuf_tensor(_uname(name), shape, dt)))


def ps(nc, st, name, shape, dt=F32):
    return Buf(st.enter_context(nc.psum_tensor(_uname(name), shape, dt)))


def fm(ap):
    return ap.rearrange("(kc p) t -> p kc t", p=128)


_CAST_BUF = [Buf(None), Buf(None)]
_CAST_N = [0]


def op_cast(S, src, dst, R, C):
    c = 2048
    while C % c:
        c //= 2
    per_row = C // c
    rows_per = max(1, 4096 // per_row)
    rows_per = max(1, 2048 // per_row)
    for r0 in range(0, R, rows_per):
        r1 = min(R, r0 + rows_per)
        S.dma("pool", lambda e, r0=r0, r1=r1: e.dma_start(
            out=dst[r0:r1, :].rearrange("r (a c) -> r a c", c=c),
            in_=src[r0:r1, :].rearrange("r (a c) -> r a c", c=c)), writes=[_CAST_BUF[_CAST_N[0] % 2]])
        _CAST_N[0] += 1


def op_norm_T(S, nc, K, xsrc, ntok, gtile_ap, hT_dst, tag):
    with ExitStack() as st:
        xt = [sb(nc, st, "%s_x%d" % (tag, i), [128, D], F32) for i in range(2)]
        junk = sb(nc, st, tag + "_junk", [128, D], F32)
        xn = [sb(nc, st, "%s_xn%d" % (tag, i), [128, D], BF16) for i in range(2)]
        ss = [sb(nc, st, "%s_ss%d" % (tag, i), [128, 2], F32) for i in range(2)]
        hTt = [sb(nc, st, "%s_hT%d" % (tag, i), [128, 16, 512], BF16) for i in range(2)]
        pT = [ps(nc, st, "%s_pT%d" % (tag, i), [128, D], BF16) for i in range(2)]
        g = None
        if gtile_ap is not None:
            g = sb(nc, st, tag + "_g", [128, D], F32)
            S.dma("sp", lambda e: e.dma_start(out=g[:], in_=gtile_ap), writes=[g])
        n = 0
        for grp in range(ntok // 512):
            hb = hTt[grp % 2]
            for s in range(4):
                i = n % 2
                t0 = grp * 512 + s * 128
                S.dma("sp", lambda e, i=i, t0=t0: e.dma_start(out=xt[i][:], in_=xsrc[t0:t0 + 128, :]), writes=[xt[i]])
                S.op("act", lambda e, i=i: e.activation(out=junk[:], in_=xt[i][:], func=AF.Square), reads=[xt[i]], writes=[junk])
                S.op("dve", lambda e, i=i: e.tensor_reduce(out=ss[i][:, 0:1], in_=junk[:], axis=AX.X, op=ALU.add), reads=[junk], writes=[ss[i]])

                S.op("act", lambda e, i=i: e.activation(out=ss[i][:, 1:2], in_=ss[i][:, 0:1], func=AF.Sqrt, scale=1.0 / D, bias=K.epsc[:, 0:1]),
                     reads=[ss[i], K.epsc], writes=[ss[i]])
                S.op("dve", lambda e, i=i: e.reciprocal(out=ss[i][:, 1:2], in_=ss[i][:, 1:2]), reads=[ss[i]], writes=[ss[i]])
                if g is not None:
                    S.op("dve", lambda e, i=i: e.scalar_tensor_tensor(out=xn[i][:], in0=xt[i][:], scalar=ss[i][:, 1:2], in1=g[:],
                                                                      op0=ALU.mult, op1=ALU.mult),
                         reads=[xt[i], ss[i], g], writes=[xn[i]])
                else:
                    S.op("dve", lambda e, i=i: e.tensor_scalar(out=xn[i][:], in0=xt[i][:], scalar1=ss[i][:, 1:2], scalar2=None, op0=ALU.mult),
                         reads=[xt[i], ss[i]], writes=[xn[i]])

                def tr(e, i=i):
                    for kc in range(16):
                        ins = e.transpose(out=pT[i][:, kc * 128:(kc + 1) * 128], in_=xn[i][:, kc * 128:(kc + 1) * 128], identity=K.identb[:])
                    return ins
                S.op("pe", tr, reads=[xn[i], K.identb], writes=[pT[i]])
                S.op("act", lambda e, i=i, s=s, hb=hb: e.activation(out=hb[:, :, s * 128:(s + 1) * 128],
                                                                    in_=pT[i][:].rearrange("p (k t) -> p k t", k=16), func=AF.Copy),
                     reads=[pT[i]], writes=[hb])
                n += 1
            S.dma("pool", lambda e, hb=hb, grp=grp: e.dma_start(out=fm(hT_dst)[:, :, grp * 512:(grp + 1) * 512], in_=hb[:]), reads=[hb])
        S.barrier()


def op_inproj(S, nc, K, L):
    hT, wb = K.hT, K.wb_in
    TS = 1536
    with ExitStack() as st:
        hTs = sb(nc, st, "ip_hTs", [128, 16, TS], BF16)
        wch = [sb(nc, st, "ip_w%d" % i, [128, 16, 512], BF16) for i in range(4)]
        pa = [ps(nc, st, "ip_pa%d" % i, [128, 512]) for i in range(2)]
        pb = [ps(nc, st, "ip_pb%d" % i, [128, 512]) for i in range(2)]
        pc = [ps(nc, st, "ip_pc%d" % i, [128, 512]) for i in range(2)]
        sq = [sb(nc, st, "ip_sq%d" % i, [128, 512], F32) for i in range(2)]
        rs = [sb(nc, st, "ip_rs%d" % i, [128, 512], F32) for i in range(2)]
        ob = [sb(nc, st, "ip_ob%d" % i, [128, 512], BF16) for i in range(3)]
        of = [sb(nc, st, "ip_of%d" % i, [128, 512], F32) for i in range(2)]
        sg = [sb(nc, st, "ip_sg%d" % i, [128, 512], F32) for i in range(2)]
        wn = [0]

        def loadw(blk):
            w = wch[wn[0] % 4]
            wn[0] += 1
            S.dma("sp", lambda e, w=w, blk=blk: e.dma_start(out=w[:], in_=fm(wb)[:, :, blk * 512:(blk + 1) * 512]), writes=[w])
            return w

        cn = [0]
        for sti in range(TE // TS):
            t_base = sti * TS
            S.dma("sp", lambda e, t_base=t_base: e.dma_start(out=hTs[:], in_=fm(hT)[:, :, t_base:t_base + TS]), writes=[hTs])

            def mm_fm(P, w, j, tt):
                def f(e):
                    for kc in range(16):
                        ins = e.matmul(P[:], w[:, kc, j * 128:(j + 1) * 128], hTs[:, kc, tt * 512:(tt + 1) * 512],
                                       start=(kc == 0), stop=(kc == 15))
                    return ins
                S.op("pe", f, reads=[w, hTs], writes=[P])

            for blk in range(4):
                w = loadw(blk)
                dst = K.qT if blk < 2 else K.kT
                gv = K.qg if blk < 2 else K.kg
                for j in range(4):
                    row0 = ((blk % 2) * 4 + j) * 128
                    for tt in range(TS // 512):
                        c = cn[0] % 2
                        cn[0] += 1
                        A, C = pa[c], pc[c]
                        mm_fm(A, w, j, tt)
                        S.op("act", lambda e, A=A, c=c: e.activation(out=sq[c][:], in_=A[:], func=AF.Square), reads=[A], writes=[sq[c]])
                        S.op("pe", lambda e, C=C, c=c: e.matmul(C[:], K.blk64[:], sq[c][:], start=True, stop=True),
                             reads=[K.blk64, sq[c]], writes=[C])

                        S.op("act", lambda e, C=C, c=c: e.activation(out=rs[c][:], in_=C[:], func=AF.Sqrt, scale=1.0 / 64, bias=K.epsc[:, 0:1]),
                             reads=[C, K.epsc], writes=[rs[c]])
                        S.op("dve", lambda e, c=c: e.reciprocal(out=rs[c][:], in_=rs[c][:]), reads=[rs[c]], writes=[rs[c]])
                        o = ob[cn[0] % 3]
                        S.op("dve", lambda e, A=A, c=c, o=o, gv=gv: e.scalar_tensor_tensor(
                            out=o[:], in0=A[:], scalar=gv[:, 0:1], in1=rs[c][:], op0=ALU.mult, op1=ALU.mult),
                            reads=[A, rs[c], gv], writes=[o])
                        t0 = t_base + tt * 512
                        S.dma("pool", lambda e, o=o, dst=dst, row0=row0, t0=t0: e.dma_start(out=dst[row0:row0 + 128, t0:t0 + 512], in_=o[:]),
                              reads=[o])
            for blk in (4, 5):
                w = loadw(blk)
                for t128 in range(TS // 128):
                    c = cn[0] % 2
                    cn[0] += 1
                    A = pa[c]

                    def f(e, A=A, w=w, t128=t128):
                        for kc in range(16):
                            ins = e.matmul(A[:], hTs[:, kc, t128 * 128:(t128 + 1) * 128], w[:, kc, :], start=(kc == 0), stop=(kc == 15))
                        return ins
                    S.op("pe", f, reads=[w, hTs], writes=[A])
                    o = ob[cn[0] % 3]
                    S.op("act", lambda e, A=A, o=o: e.activation(out=o[:], in_=A[:], func=AF.Copy), reads=[A], writes=[o])
                    t0 = t_base + t128 * 128
                    S.dma("pool", lambda e, o=o, t0=t0, blk=blk: e.dma_start(out=K.v[t0:t0 + 128, (blk - 4) * 512:(blk - 3) * 512], in_=o[:]),
                          reads=[o])
            for u in range(2):
                wa = loadw(6 + u)
                wb_ = loadw(8 + u)
                for j in range(4):
                    row0 = (u * 4 + j) * 128
                    for tt in range(TS // 512):
                        c = cn[0] % 2
                        cn[0] += 1
                        A, B = pa[c], pb[c]
                        mm_fm(A, wa, j, tt)
                        mm_fm(B, wb_, j, tt)
                        S.op("act", lambda e, B=B, c=c: e.activation(out=sg[c][:], in_=B[:], func=AF.Sigmoid), reads=[B], writes=[sg[c]])
                        S.op("dve", lambda e, A=A, c=c: e.tensor_tensor(out=of[c][:], in0=A[:], in1=sg[c][:], op=ALU.mult),
                             reads=[A, sg[c]], writes=[of[c]])
                        t0 = t_base + tt * 512
                        S.dma("pool", lambda e, c=c, row0=row0, t0=t0: e.dma_start(out=K.gluT[row0:row0 + 128, t0:t0 + 512], in_=of[c][:]),
                              reads=[of[c]])
            for blk in range(10, 18):
                w = loadw(blk)
                dst = K.gaT if blk < 14 else K.gcT
                for j in range(4):
                    row0 = (((blk - 10) % 4) * 4 + j) * 128
                    for tt in range(TS // 512):
                        c = cn[0] % 2
                        cn[0] += 1
                        A = pa[c]
                        mm_fm(A, w, j, tt)
                        o = ob[cn[0] % 3]
                        S.op("act", lambda e, A=A, o=o: e.activation(out=o[:], in_=A[:], func=AF.Sigmoid), reads=[A], writes=[o])
                        t0 = t_base + tt * 512
                        S.dma("pool", lambda e, o=o, dst=dst, row0=row0, t0=t0: e.dma_start(out=dst[row0:row0 + 128, t0:t0 + 512], in_=o[:]),
                              reads=[o])
        S.barrier()


def op_attention(S, nc, K, L):
    with ExitStack() as st:
        kth = [sb(nc, st, "at_k%d" % i, [64, TE], BF16) for i in range(2)]
        qth = [sb(nc, st, "at_q%d" % i, [64, T], BF16) for i in range(2)]
        vh = [sb(nc, st, "at_v%d" % i, [128, TE // 128, 64], BF16) for i in range(2)]
        tab = [sb(nc, st, "at_tab%d" % i, [128, 3, ATT_COLS], F32) for i in range(2)]
        sT = [ps(nc, st, "at_s%d" % i, [128, 512]) for i in range(2)]
        pv = [ps(nc, st, "at_pv%d" % i, [64, 512]) for i in range(2)]
        pz = [ps(nc, st, "at_pz%d" % i, [64, 512]) for i in range(2)]
        sf = [sb(nc, st, "at_sf%d" % i, [128, 512], F32) for i in range(2)]
        pe_ = [sb(nc, st, "at_p%d" % i, [128, 512], BF16) for i in range(3)]
        rz = [sb(nc, st, "at_rz%d" % i, [64, 512], F32) for i in range(2)]
        ao = [sb(nc, st, "at_ao%d" % i, [64, 512], BF16) for i in range(2)]
        coff = []
        o = 0
        for a, b in ATT_RNG:
            coff.append(o)
            o += (b - a + 1) * 64
        n = 0
        for h in range(16):
            hb = h % 2
            r0 = h * 64
            S.dma("sp", lambda e, hb=hb, r0=r0: e.dma_start(out=kth[hb][:], in_=K.kT[r0:r0 + 64, :]), writes=[kth[hb]])
            S.dma("sp", lambda e, hb=hb, r0=r0: e.dma_start(out=qth[hb][:], in_=K.qT[r0:r0 + 64, HALO:HALO + T]), writes=[qth[hb]])
            with nc.allow_non_contiguous_dma(reason="per-head V slice"):
                S.dma("sp", lambda e, hb=hb, r0=r0: e.dma_start(out=vh[hb][:], in_=K.v.rearrange("(c p) n -> p c n", p=128)[:, :, r0:r0 + 64]),
                      writes=[vh[hb]])
            S.dma("sp", lambda e, hb=hb, h=h: e.dma_start(out=tab[hb][:], in_=K.att_tab[L, h]), writes=[tab[hb]])
            for blk in range(8):
                slot = 0 if blk == 0 else (2 if blk == 7 else 1)
                PV, PZ = pv[blk % 2], pz[blk % 2]
                first = True
                for oi, ci in enumerate(ATT_ORDER):
                    last = oi == len(ATT_ORDER) - 1
                    a, b = ATT_RNG[ci]
                    ncol = (b - a + 1) * 64
                    q0 = blk * 512 + a * 64
                    kt0 = blk * 512 + ci * 128
                    c = n % 2
                    p3 = pe_[n % 3]
                    n += 1
                    S.op("pe", lambda e, c=c, hb=hb, kt0=kt0, q0=q0, ncol=ncol: e.matmul(
                        sT[c][:, 0:ncol], kth[hb][:, kt0:kt0 + 128], qth[hb][:, q0:q0 + ncol], start=True, stop=True),
                        reads=[kth[hb], qth[hb]], writes=[sT[c]])
                    S.op("dve", lambda e, c=c, hb=hb, slot=slot, ci=ci, ncol=ncol: e.tensor_tensor(
                        out=sf[c][:, 0:ncol], in0=sT[c][:, 0:ncol], in1=tab[hb][:, slot, coff[ci]:coff[ci] + ncol], op=ALU.add),
                        reads=[sT[c], tab[hb]], writes=[sf[c]])
                    S.op("act", lambda e, c=c, p3=p3, ncol=ncol: e.activation(out=p3[:, 0:ncol], in_=sf[c][:, 0:ncol], func=AF.Exp),
                         reads=[sf[c]], writes=[p3])
                    kc = kt0 // 128

                    def pvf(e, p3=p3, hb=hb, kc=kc, a=a, ncol=ncol, first=first, last=last, PV=PV, PZ=PZ):
                        e.matmul(PV[:, a * 64:a * 64 + ncol], vh[hb][:, kc, :], p3[:, 0:ncol], start=first, stop=last, skip_group_check=True)
                        return e.matmul(PZ[:, a * 64:a * 64 + ncol], K.ones_b[:, 0:64], p3[:, 0:ncol], start=first, stop=last,
                                        skip_group_check=True)
                    S.op("pe", pvf, reads=[p3, vh[hb], K.ones_b], writes=[PV, PZ])
                    first = False
                i2 = blk % 2
                S.op("dve", lambda e, i2=i2, PZ=PZ: e.reciprocal(out=rz[i2][:], in_=PZ[:]), reads=[PZ], writes=[rz[i2]])
                S.op("dve", lambda e, i2=i2, PV=PV: e.tensor_tensor(out=ao[i2][:], in0=PV[:], in1=rz[i2][:], op=ALU.mult),
                     reads=[PV, rz[i2]], writes=[ao[i2]])
                S.dma("pool", lambda e, i2=i2, r0=r0, blk=blk: e.dma_start(out=K.attT[r0:r0 + 64, blk * 512:(blk + 1) * 512], in_=ao[i2][:]),
                      reads=[ao[i2]])
        S.barrier()


def op_conv(S, nc, K, L):
    with ExitStack() as st:
        gin = [sb(nc, st, "cv_in%d" % i, [128, 542], F32) for i in range(3)]
        ck = [sb(nc, st, "cv_ck%d" % i, [128, 512], F32) for i in range(8)]
        sq = [sb(nc, st, "cv_sq%d" % i, [128, 512], F32) for i in range(2)]
        pm = ps(nc, st, "cv_pm", [128, 512])
        pq = ps(nc, st, "cv_pq", [128, 512])
        mean = sb(nc, st, "cv_mean", [128, 512], F32)
        rstd = sb(nc, st, "cv_rstd", [128, 512], F32)
        tmp = [sb(nc, st, "cv_tmp%d" % i, [128, 512], F32) for i in range(2)]
        ob = [sb(nc, st, "cv_ob%d" % i, [128, 512], BF16) for i in range(2)]
        n = 0
        for tt in range(T // 512):
            t0 = HALO + tt * 512 - 15
            for cc in range(8):
                g = gin[n % 3]
                n += 1
                S.dma("sp", lambda e, g=g, cc=cc, t0=t0: e.dma_start(out=g[:], in_=K.gluT[cc * 128:(cc + 1) * 128, t0:t0 + 542]), writes=[g])
                eng = "dve"
                acc = ck[cc]

                fns = [lambda e, g=g, cc=cc, acc=acc: e.tensor_scalar(out=acc[:], in0=g[:, 0:512], scalar1=K.wdw[:, L, cc, 0:1],
                                                                      scalar2=K.cvp[:, L, 0, cc:cc + 1], op0=ALU.mult, op1=ALU.add)]
                for k in range(1, 31):
                    fns.append(lambda e, g=g, cc=cc, acc=acc, k=k: e.scalar_tensor_tensor(
                        out=acc[:], in0=g[:, k:k + 512], scalar=K.wdw[:, L, cc, k:k + 1], in1=acc[:], op0=ALU.mult, op1=ALU.add))
                chain(S, eng, fns, reads=[g, K.wdw, K.cvp], writes=[acc])
                s2 = sq[cc % 2]
                S.op("act", lambda e, s2=s2, acc=acc: e.activation(out=s2[:], in_=acc[:], func=AF.Square), reads=[acc], writes=[s2])
                S.op("pe", lambda e, acc=acc, cc=cc: e.matmul(pm[:], K.ones_f[:], acc[:], start=(cc == 0), stop=(cc == 7)),
                     reads=[acc, K.ones_f], writes=[pm])
                S.op("pe", lambda e, s2=s2, cc=cc: e.matmul(pq[:], K.ones_f[:], s2[:], start=(cc == 0), stop=(cc == 7)),
                     reads=[s2, K.ones_f], writes=[pq])

            chain(S, "dve", [
                lambda e: e.tensor_scalar(out=mean[:], in0=pm[:], scalar1=1.0 / 1024, scalar2=None, op0=ALU.mult),
                lambda e: e.tensor_tensor(out=rstd[:], in0=mean[:], in1=mean[:], op=ALU.mult),
                lambda e: e.scalar_tensor_tensor(out=rstd[:], in0=pq[:], scalar=1.0 / 1024, in1=rstd[:], op0=ALU.mult, op1=ALU.subtract),
            ], reads=[pm, pq], writes=[mean, rstd])
            S.op("act", lambda e: e.activation(out=rstd[:], in_=rstd[:], func=AF.Sqrt, bias=K.epsc[:, 0:1]), reads=[rstd, K.epsc], writes=[rstd])
            S.op("dve", lambda e: e.reciprocal(out=rstd[:], in_=rstd[:]), reads=[rstd], writes=[rstd])
            for cc in range(8):
                tm = tmp[cc % 2]
                o = ob[cc % 2]
                acc = ck[cc]

                chain(S, "dve", [
                    lambda e, tm=tm, acc=acc: e.tensor_tensor(out=tm[:], in0=acc[:], in1=mean[:], op=ALU.subtract),
                    lambda e, tm=tm: e.tensor_tensor(out=tm[:], in0=tm[:], in1=rstd[:], op=ALU.mult),
                    lambda e, tm=tm, cc=cc: e.tensor_scalar(out=tm[:], in0=tm[:], scalar1=K.cvp[:, L, 1, cc:cc + 1],
                                                            scalar2=K.cvp[:, L, 2, cc:cc + 1], op0=ALU.mult, op1=ALU.add),
                ], reads=[acc, mean, rstd, K.cvp], writes=[tm])
                S.op("act", lambda e, tm=tm, o=o: e.activation(out=o[:], in_=tm[:], func=AF.Silu), reads=[tm], writes=[o])
                S.dma("pool", lambda e, o=o, cc=cc, tt=tt: e.dma_start(out=K.convT[cc * 128:(cc + 1) * 128, tt * 512:(tt + 1) * 512], in_=o[:]),
                      reads=[o])
        S.barrier()


def op_mix(S, nc, K, L):
    with ExitStack() as st:
        wa = sb(nc, st, "mx_wa", [128, 8, D], BF16)
        wc = sb(nc, st, "mx_wc", [128, 8, D], BF16)
        at = [sb(nc, st, "mx_at%d" % i, [128, 8, 512], BF16) for i in range(2)]
        cv = [sb(nc, st, "mx_cv%d" % i, [128, 8, 512], BF16) for i in range(2)]
        ga = [sb(nc, st, "mx_ga%d" % i, [128, 16, 512], BF16) for i in range(2)]
        gc = [sb(nc, st, "mx_gc%d" % i, [128, 16, 512], BF16) for i in range(2)]
        mt = [sb(nc, st, "mx_mt%d" % i, [128, 16, 512], BF16) for i in range(2)]
        t1 = [sb(nc, st, "mx_t1%d" % i, [128, 512], F32) for i in range(2)]
        t2 = [sb(nc, st, "mx_t2%d" % i, [128, 512], F32) for i in range(2)]
        pa = [ps(nc, st, "mx_pa%d" % i, [128, 512]) for i in range(2)]
        pb = [ps(nc, st, "mx_pb%d" % i, [128, 512]) for i in range(2)]
        S.dma("sp", lambda e: e.dma_start(out=wa[:], in_=fm(K.wb_ao)), writes=[wa])
        S.dma("sp", lambda e: e.dma_start(out=wc[:], in_=fm(K.wb_co)), writes=[wc])
        n = 0
        for tt in range(T // 512):
            i = tt % 2
            S.dma("sp", lambda e, i=i, tt=tt: e.dma_start(out=at[i][:], in_=fm(K.attT)[:, :, tt * 512:(tt + 1) * 512]), writes=[at[i]])
            S.dma("sp", lambda e, i=i, tt=tt: e.dma_start(out=cv[i][:], in_=fm(K.convT)[:, :, tt * 512:(tt + 1) * 512]), writes=[cv[i]])
            S.dma("sp", lambda e, i=i, tt=tt: e.dma_start(out=ga[i][:], in_=fm(K.gaT)[:, :, HALO + tt * 512:HALO + (tt + 1) * 512]), writes=[ga[i]])
            S.dma("sp", lambda e, i=i, tt=tt: e.dma_start(out=gc[i][:], in_=fm(K.gcT)[:, :, HALO + tt * 512:HALO + (tt + 1) * 512]), writes=[gc[i]])
            for oc in range(16):
                c = n % 2
                n += 1

                def f(e, c=c, i=i, oc=oc):
                    for kc in range(8):
                        e.matmul(pa[c][:], wa[:, kc, oc * 128:(oc + 1) * 128], at[i][:, kc, :], start=(kc == 0), stop=(kc == 7))
                    for kc in range(8):
                        ins = e.matmul(pb[c][:], wc[:, kc, oc * 128:(oc + 1) * 128], cv[i][:, kc, :], start=(kc == 0), stop=(kc == 7))
                    return ins
                S.op("pe", f, reads=[wa, wc, at[i], cv[i]], writes=[pa[c], pb[c]])
                S.op("dve", lambda e, c=c, i=i, oc=oc: e.tensor_tensor(out=t1[c][:], in0=pa[c][:], in1=ga[i][:, oc, :], op=ALU.mult),
                     reads=[pa[c], ga[i]], writes=[t1[c]])
                S.op("dve", lambda e, c=c, i=i, oc=oc: e.tensor_tensor(out=t2[c][:], in0=pb[c][:], in1=gc[i][:, oc, :], op=ALU.mult),
                     reads=[pb[c], gc[i]], writes=[t2[c]])
                S.op("pool", lambda e, c=c, i=i, oc=oc: e.tensor_tensor(out=mt[i][:, oc, :], in0=t1[c][:], in1=t2[c][:], op=ALU.add),
                     reads=[t1[c], t2[c]], writes=[mt[i]])
            S.dma("pool", lambda e, i=i, tt=tt: e.dma_start(out=fm(K.mT)[:, :, tt * 512:(tt + 1) * 512], in_=mt[i][:]), reads=[mt[i]])
        S.barrier()


def op_resid_linear(S, nc, K, inT, wbf, kchunks, xsrc, xsrc_off, xdst, tag):
    with ExitStack() as st:
        w = sb(nc, st, tag + "_w", [128, kchunks, D], BF16)
        it = [sb(nc, st, "%s_it%d" % (tag, i), [128, kchunks, 512], BF16) for i in range(2)]
        xt = [sb(nc, st, "%s_xt%d" % (tag, i), [128, D], F32) for i in range(2)]
        pp = [ps(nc, st, "%s_p%d" % (tag, i), [128, 512]) for i in range(4)]
        S.dma("sp", lambda e: e.dma_start(out=w[:], in_=fm(wbf)), writes=[w])
        n = 0
        for tt in range(T // 512):
            i = tt % 2
            S.dma("sp", lambda e, i=i, tt=tt: e.dma_start(out=it[i][:], in_=fm(inT)[:, :, tt * 512:(tt + 1) * 512]), writes=[it[i]])
            for s in range(4):
                x = xt[n % 2]
                n += 1
                t0 = tt * 512 + s * 128
                S.dma("sp", lambda e, x=x, t0=t0: e.dma_start(out=x[:], in_=xsrc[xsrc_off + t0:xsrc_off + t0 + 128, :]), writes=[x])
                for nb in range(4):
                    P = pp[nb]

                    def f(e, P=P, i=i, s=s, nb=nb):
                        for kc in range(kchunks):
                            ins = e.matmul(P[:], it[i][:, kc, s * 128:(s + 1) * 128], w[:, kc, nb * 512:(nb + 1) * 512],
                                           start=(kc == 0), stop=(kc == kchunks - 1))
                        return ins
                    S.op("pe", f, reads=[it[i], w], writes=[P])
                    S.op("dve", lambda e, P=P, x=x, nb=nb: e.tensor_tensor(out=x[:, nb * 512:(nb + 1) * 512], in0=P[:],
                                                                           in1=x[:, nb * 512:(nb + 1) * 512], op=ALU.add),
                         reads=[P, x], writes=[x])
                S.dma("pool", lambda e, x=x, t0=t0: e.dma_start(out=xdst[t0:t0 + 128, :], in_=x[:]), reads=[x])
        S.barrier()


def op_post(S, nc, K, L):
    with ExitStack() as st:
        xt = [sb(nc, st, "po_x%d" % i, [128, D], F32) for i in range(2)]
        junk = sb(nc, st, "po_junk", [128, D], F32)
        hf = [sb(nc, st, "po_hf%d" % i, [128, D], F32) for i in range(2)]
        hb = [sb(nc, st, "po_hb%d" % i, [128, D], BF16) for i in range(2)]
        ss = [sb(nc, st, "po_ss%d" % i, [128, 2], F32) for i in range(2)]
        g = sb(nc, st, "po_g", [128, D], F32)
        wr = sb(nc, st, "po_wr", [128, 16, 16], F32)
        hT = [sb(nc, st, "po_hT%d" % i, [128, 16, 128], F32) for i in range(2)]
        pt = [ps(nc, st, "po_pt%d" % i, [128, 512]) for i in range(2)]
        pl = [ps(nc, st, "po_pl%d" % i, [128, 16]) for i in range(2)]
        pa = ps(nc, st, "po_pa", [16, 128])
        sm = [sb(nc, st, "po_sm%d" % i, [128, 20], F32) for i in range(2)]
        af = [sb(nc, st, "po_af%d" % i, [128, 16], F32) for i in range(2)]
        affT = sb(nc, st, "po_affT", [16, T], F32)
        S.dma("sp", lambda e: e.dma_start(out=g[:], in_=K.gains[L, 1]), writes=[g])
        S.dma("sp", lambda e: e.dma_start(out=wr[:], in_=K.w_router[L].rearrange("(kc p) e -> p kc e", p=128)), writes=[wr])
        for tt in range(T // 128):
            i = tt % 2
            t0 = tt * 128
            S.dma("sp", lambda e, i=i, t0=t0: e.dma_start(out=xt[i][:], in_=K.x1[t0:t0 + 128, :]), writes=[xt[i]])
            S.op("act", lambda e, i=i: e.activation(out=junk[:], in_=xt[i][:], func=AF.Square), reads=[xt[i]], writes=[junk])
            S.op("dve", lambda e, i=i: e.tensor_reduce(out=ss[i][:, 0:1], in_=junk[:], axis=AX.X, op=ALU.add), reads=[junk], writes=[ss[i]])
            S.op("act", lambda e, i=i: e.activation(out=ss[i][:, 1:2], in_=ss[i][:, 0:1], func=AF.Sqrt, scale=1.0 / D, bias=K.epsc[:, 0:1]),
                 reads=[ss[i], K.epsc], writes=[ss[i]])
            S.op("dve", lambda e, i=i: e.reciprocal(out=ss[i][:, 1:2], in_=ss[i][:, 1:2]), reads=[ss[i]], writes=[ss[i]])
            S.op("dve", lambda e, i=i: e.scalar_tensor_tensor(out=hf[i][:], in0=xt[i][:], scalar=ss[i][:, 1:2], in1=g[:], op0=ALU.mult, op1=ALU.mult),
                 reads=[xt[i], ss[i], g], writes=[hf[i]])
            S.op("pool", lambda e, i=i: e.tensor_copy(out=hb[i][:], in_=hf[i][:]), reads=[hf[i]], writes=[hb[i]])
            S.dma("pool", lambda e, i=i, t0=t0: e.dma_start(out=K.h2own[t0:t0 + 128, :], in_=hb[i][:]), reads=[hb[i]])
            for q4 in range(4):
                P = pt[q4 % 2]

                def tr(e, P=P, i=i, q4=q4):
                    for k in range(4):
                        kc = q4 * 4 + k
                        ins = e.transpose(out=P[:, k * 128:(k + 1) * 128], in_=hf[i][:, kc * 128:(kc + 1) * 128], identity=K.identf[:])
                    return ins
                S.op("pe", tr, reads=[hf[i], K.identf], writes=[P])
                S.op("act", lambda e, P=P, i=i, q4=q4: e.activation(out=hT[i][:, q4 * 4:(q4 + 1) * 4, :],
                                                                    in_=P[:].rearrange("p (k t) -> p k t", k=4), func=AF.Copy),
                     reads=[P], writes=[hT[i]])

            def lg(e, i=i):
                for kc in range(16):
                    ins = e.matmul(pl[i][:], hT[i][:, kc, :], wr[:, kc, :], start=(kc == 0), stop=(kc == 15))
                return ins
            S.op("pe", lg, reads=[hT[i], wr], writes=[pl[i]])
            S.op("dve", lambda e, i=i: e.tensor_reduce(out=sm[i][:, 16:17], in_=pl[i][:], axis=AX.X, op=ALU.max), reads=[pl[i]], writes=[sm[i]])
            S.op("dve", lambda e, i=i: e.tensor_scalar(out=sm[i][:, 17:18], in0=sm[i][:, 16:17], scalar1=-1.0, scalar2=None, op0=ALU.mult),
                 reads=[sm[i]], writes=[sm[i]])
            S.op("act", lambda e, i=i: e.activation(out=sm[i][:, 0:16], in_=pl[i][:], func=AF.Exp, bias=sm[i][:, 17:18]),
                 reads=[pl[i], sm[i]], writes=[sm[i]])
            S.op("dve", lambda e, i=i: e.tensor_reduce(out=sm[i][:, 18:19], in_=sm[i][:, 0:16], axis=AX.X, op=ALU.add), reads=[sm[i]], writes=[sm[i]])
            S.op("dve", lambda e, i=i: e.reciprocal(out=sm[i][:, 19:20], in_=sm[i][:, 18:19]), reads=[sm[i]], writes=[sm[i]])
            S.op("dve", lambda e, i=i: e.tensor_scalar(out=af[i][:], in0=sm[i][:, 0:16], scalar1=sm[i][:, 19:20], scalar2=None, op0=ALU.mult),
                 reads=[sm[i]], writes=[af[i]])
            S.op("pe", lambda e, i=i: e.transpose(out=pa[:], in_=af[i][:], identity=K.identf[:]), reads=[af[i], K.identf], writes=[pa])
            S.op("act", lambda e, t0=t0: e.activation(out=affT[:, t0:t0 + 128], in_=pa[:], func=AF.Copy), reads=[pa], writes=[affT])
        S.dma("sp", lambda e: e.dma_start(out=K.affT_own.rearrange("(e c) t -> e (c t)", c=32), in_=affT[:]), reads=[affT])
        S.barrier()


def op_route(S, nc, K, L, st):
    A = sb(nc, st, "rt_A", [128, 4, 128], F32)
    cmpb = sb(nc, st, "rt_cmp", [128, 4, 128], F32)
    cum = sb(nc, st, "rt_cum", [128, 4, 128], F32)
    ones3 = sb(nc, st, "rt_ones", [128, 128], F32)
    sm = sb(nc, st, "rt_sm", [128, 8, 4], F32)
    pc = ps(nc, st, "rt_pc", [128, 4])
    rtab = sb(nc, st, "rt_rtab", [128, 4, 128, 6], BF16)
    r1 = sb(nc, st, "rt_r1", [128, 4, 128], F32)
    hi_f = sb(nc, st, "rt_hif", [128, 4, 128], F32)
    sel = [sb(nc, st, "rt_sel%d" % i, [128, CAP], BF16) for i in range(3)]
    pidx = [ps(nc, st, "rt_pidx%d" % i, [128, 16, 6]) for i in range(2)]
    res = sb(nc, st, "rt_res", [128, 16, 6], F32)
    idxf = sb(nc, st, "rt_idxf", [128, 16], F32)
    LO, HI, MID, CNT, GE, T1, T2, BASE = range(8)
    for j in range(4):
        S.dma("pool", lambda e, j=j: e.indirect_dma_start(
            out=A[:, j, :], out_offset=None, in_=K.affT_all,
            in_offset=bass.IndirectOffsetOnAxis(ap=K.ridx[:, j:j + 1], axis=0)), reads=[K.ridx], writes=[A])
    S.op("dve", lambda e: e.memset(ones3[:], 1.0), writes=[ones3])
    S.op("dve", lambda e: e.memset(sm[:, LO, :], 0.0), writes=[sm])
    S.op("dve", lambda e: e.memset(sm[:, HI, :], 1.0), writes=[sm])

    def count_ge(col):
        for j in range(4):
            S.op("dve", lambda e, j=j: e.tensor_scalar(out=cmpb[:, j, :], in0=A[:, j, :], scalar1=sm[:, col, j:j + 1], scalar2=None, op0=ALU.is_ge),
                 reads=[A, sm], writes=[cmpb])

    for it in range(36):
        S.op("dve", lambda e: e.tensor_tensor(out=sm[:, MID, :], in0=sm[:, LO, :], in1=sm[:, HI, :], op=ALU.add), reads=[sm], writes=[sm])
        S.op("dve", lambda e: e.tensor_scalar(out=sm[:, MID, :], in0=sm[:, MID, :], scalar1=0.5, scalar2=None, op0=ALU.mult), reads=[sm], writes=[sm])
        count_ge(MID)
        S.op("dve", lambda e: e.tensor_reduce(out=sm[:, CNT, :], in_=cmpb[:], axis=AX.X, op=ALU.add), reads=[cmpb], writes=[sm])
        S.op("pe", lambda e: e.matmul(pc[:], K.ones_f[:], sm[:, CNT, :], start=True, stop=True), reads=[K.ones_f, sm], writes=[pc])
        S.op("dve", lambda e: e.tensor_scalar(out=sm[:, GE, :], in0=pc[:], scalar1=float(CAP) - 0.5, scalar2=None, op0=ALU.is_ge), reads=[pc], writes=[sm])
        S.op("dve", lambda e: e.tensor_tensor(out=sm[:, T1, :], in0=sm[:, GE, :], in1=sm[:, MID, :], op=ALU.mult), reads=[sm], writes=[sm])
        S.op("dve", lambda e: e.tensor_tensor(out=sm[:, LO, :], in0=sm[:, LO, :], in1=sm[:, T1, :], op=ALU.max), reads=[sm], writes=[sm])
        S.op("dve", lambda e: e.tensor_scalar(out=sm[:, T2, :], in0=sm[:, GE, :], scalar1=2.0, scalar2=None, op0=ALU.mult), reads=[sm], writes=[sm])
        S.op("dve", lambda e: e.tensor_tensor(out=sm[:, T2, :], in0=sm[:, T2, :], in1=sm[:, MID, :], op=ALU.add), reads=[sm], writes=[sm])
        S.op("dve", lambda e: e.tensor_tensor(out=sm[:, HI, :], in0=sm[:, HI, :], in1=sm[:, T2, :], op=ALU.min), reads=[sm], writes=[sm])
    count_ge(LO)
    for j in range(4):
        S.op("dve", lambda e, j=j: e.tensor_tensor_scan(out=cum[:, j, :], data0=ones3[:], data1=cmpb[:, j, :], initial=0.0, op0=ALU.mult, op1=ALU.add),
             reads=[ones3, cmpb], writes=[cum])
    S.op("dve", lambda e: e.tensor_copy(out=sm[:, CNT, :], in_=cum[:, :, 127]), reads=[cum], writes=[sm])
    S.op("pe", lambda e: e.matmul(pc[:], K.tri[:], sm[:, CNT, :], start=True, stop=True), reads=[K.tri, sm], writes=[pc])
    S.op("dve", lambda e: e.tensor_copy(out=sm[:, BASE, :], in_=pc[:]), reads=[pc], writes=[sm])
    for j in range(4):
        S.op("dve", lambda e, j=j: e.tensor_scalar(out=cum[:, j, :], in0=cum[:, j, :], scalar1=sm[:, BASE, j:j + 1], scalar2=None, op0=ALU.add),
             reads=[cum, sm], writes=[cum])
        S.op("dve", lambda e, j=j: e.tensor_tensor(out=cum[:, j, :], in0=cum[:, j, :], in1=cmpb[:, j, :], op=ALU.mult), reads=[cum, cmpb], writes=[cum])
        S.op("dve", lambda e, j=j: e.tensor_scalar(out=cum[:, j, :], in0=cum[:, j, :], scalar1=-1.0, scalar2=None, op0=ALU.add), reads=[cum], writes=[cum])
    S.dma("pool", lambda e: e.dma_start(out=rtab[:].rearrange("p a b c -> p a (b c)"), in_=K.rtab_in.rearrange("p a b c -> p a (b c)")),
          writes=[rtab])
    S.op("dve", lambda e: e.tensor_copy(out=rtab[:, :, :, 3], in_=A[:]), reads=[A], writes=[rtab])
    S.op("dve", lambda e: e.tensor_copy(out=hi_f[:], in_=rtab[:, :, :, 3]), reads=[rtab], writes=[hi_f])
    S.op("dve", lambda e: e.tensor_tensor(out=r1[:], in0=A[:], in1=hi_f[:], op=ALU.subtract), reads=[A, hi_f], writes=[r1])
    S.op("dve", lambda e: e.tensor_copy(out=rtab[:, :, :, 4], in_=r1[:]), reads=[r1], writes=[rtab])
    S.op("dve", lambda e: e.tensor_copy(out=hi_f[:], in_=rtab[:, :, :, 4]), reads=[rtab], writes=[hi_f])
    S.op("dve", lambda e: e.tensor_tensor(out=r1[:], in0=r1[:], in1=hi_f[:], op=ALU.subtract), reads=[r1, hi_f], writes=[r1])
    S.op("dve", lambda e: e.tensor_copy(out=rtab[:, :, :, 5], in_=r1[:]), reads=[r1], writes=[rtab])
    n = 0
    for j in range(4):
        P = pidx[j % 2]
        for jj in range(128):
            sl = sel[n % 3]
            eng = "pool" if n % 3 == 2 else "dve"
            n += 1
            S.op(eng, lambda e, sl=sl, j=j, jj=jj: e.tensor_scalar(out=sl[:], in0=K.iota_s[:], scalar1=cum[:, j, jj:jj + 1], scalar2=None, op0=ALU.is_equal),
                 reads=[K.iota_s, cum], writes=[sl])

            def mm(e, sl=sl, j=j, jj=jj, P=P):
                for c in range(16):
                    ins = e.matmul(P[:, c, :], sl[:, c * 128:(c + 1) * 128], rtab[:, j, jj, :], start=(jj == 0 and c == 0),
                                   stop=(jj == 127 and c == 15), skip_group_check=True)
                return ins
            S.op("pe", mm, reads=[sl, rtab], writes=[P])
        S.op("act", lambda e, P=P: e.activation(out=res[:], in_=P[:], func=AF.Copy), reads=[P], writes=[res])
        S.op("dve", lambda e: e.tensor_tensor(out=idxf[:], in0=res[:, :, 0], in1=res[:, :, 2], op=ALU.add), reads=[res], writes=[idxf])
        S.op("dve", lambda e, j=j: e.tensor_copy(out=K.idx[:, j, :], in_=idxf[:]), reads=[idxf], writes=[K.idx])
        S.op("dve", lambda e: e.tensor_tensor(out=idxf[:], in0=res[:, :, 1], in1=res[:, :, 2], op=ALU.add), reads=[res], writes=[idxf])
        S.op("dve", lambda e, j=j: e.tensor_copy(out=K.idx_d[:, j, :], in_=idxf[:]), reads=[idxf], writes=[K.idx_d])
        S.op("dve", lambda e, j=j: e.tensor_tensor(out=K.gate[:, j, :], in0=res[:, :, 3], in1=res[:, :, 4], op=ALU.add), reads=[res], writes=[K.gate])
        S.op("dve", lambda e, j=j: e.tensor_tensor(out=K.gate[:, j, :], in0=K.gate[:, j, :], in1=res[:, :, 5], op=ALU.add), reads=[res, K.gate], writes=[K.gate])


def op_zero_delta(S, nc, K, L, st):
    zt = sb(nc, st, "ex_z", [128, D], F32)
    S.op("dve", lambda e: e.memset(zt[:], 0.0), writes=[zt])
    for r in range(4 * T // 128):
        S.dma("sp", lambda e, r=r: e.dma_start(out=K.delta[L][r * 128:(r + 1) * 128, :], in_=zt[:]), reads=[zt])


def op_expert(S, nc, K, L, st):
    xg = [sb(nc, st, "ex_xg%d" % i, [128, D], BF16) for i in range(3)]
    xeT = sb(nc, st, "ex_xeT", [128, 16, 512], BF16)
    hidT = sb(nc, st, "ex_hidT", [128, 16, 512], BF16)
    w1b = [sb(nc, st, "ex_w1%d" % i, [128, 16, 256], BF16) for i in range(2)]
    w3b = [sb(nc, st, "ex_w3%d" % i, [128, 16, 256], BF16) for i in range(2)]
    w2f = sb(nc, st, "ex_w2f", [128, 16, D], BF16)
    sa = [sb(nc, st, "ex_sa%d" % i, [128, 512], F32) for i in range(2)]
    ysb = [sb(nc, st, "ex_y%d" % i, [128, D], F32) for i in range(2)]
    pT = ps(nc, st, "ex_pT", [128, D], BF16)
    pa = [ps(nc, st, "ex_pa%d" % i, [128, 512]) for i in range(2)]
    pb = [ps(nc, st, "ex_pb%d" % i, [128, 512]) for i in range(2)]
    py = [ps(nc, st, "ex_py%d" % i, [128, 512]) for i in range(2)]
    delta = K.delta[L]
    ng, nw, ny, nc2 = 0, 0, 0, 0
    for j in range(4):
        S.dma("sp", lambda e, j=j: e.dma_start(out=w2f[:], in_=fm(K.wb_2[j])), writes=[w2f])
        for rt in range(4):
            for s in range(4):
                c16 = rt * 4 + s
                x = xg[ng % 3]
                ng += 1
                S.dma("pool", lambda e, x=x, j=j, c16=c16: e.indirect_dma_start(
                    out=x[:], out_offset=None, in_=K.h2_all[L],
                    in_offset=bass.IndirectOffsetOnAxis(ap=K.idx[:, j, c16:c16 + 1], axis=0)), reads=[K.idx], writes=[x])

                def tr(e, x=x):
                    for kc in range(16):
                        ins = e.transpose(out=pT[:, kc * 128:(kc + 1) * 128], in_=x[:, kc * 128:(kc + 1) * 128], identity=K.identb[:])
                    return ins
                S.op("pe", tr, reads=[x, K.identb], writes=[pT])
                S.op("act", lambda e, s=s: e.activation(out=xeT[:, :, s * 128:(s + 1) * 128], in_=pT[:].rearrange("p (k t) -> p k t", k=16),
                                                        func=AF.Copy), reads=[pT], writes=[xeT])
            for fb in range(8):
                wi = nw % 2
                nw += 1
                S.dma("sp", lambda e, wi=wi, j=j, fb=fb: e.dma_start(out=w1b[wi][:], in_=fm(K.wb_1[j])[:, :, fb * 256:(fb + 1) * 256]), writes=[w1b[wi]])
                S.dma("sp", lambda e, wi=wi, j=j, fb=fb: e.dma_start(out=w3b[wi][:], in_=fm(K.wb_3[j])[:, :, fb * 256:(fb + 1) * 256]), writes=[w3b[wi]])
                for f2 in range(2):
                    fc = fb * 2 + f2
                    c = fc % 2

                    def mm(e, c=c, wi=wi, f2=f2):
                        for kc in range(16):
                            e.matmul(pa[c][:], w1b[wi][:, kc, f2 * 128:(f2 + 1) * 128], xeT[:, kc, :], start=(kc == 0), stop=(kc == 15))
                        for kc in range(16):
                            ins = e.matmul(pb[c][:], w3b[wi][:, kc, f2 * 128:(f2 + 1) * 128], xeT[:, kc, :], start=(kc == 0), stop=(kc == 15))
                        return ins
                    S.op("pe", mm, reads=[w1b[wi], w3b[wi], xeT], writes=[pa[c], pb[c]])
                    S.op("act", lambda e, c=c: e.activation(out=sa[c][:], in_=pa[c][:], func=AF.Silu), reads=[pa[c]], writes=[sa[c]])
                    S.op("dve", lambda e, c=c, fc=fc: e.tensor_tensor(out=hidT[:, fc, :], in0=pb[c][:], in1=sa[c][:], op=ALU.mult),
                         reads=[pb[c], sa[c]], writes=[hidT])
            for s in range(4):
                ys = ysb[ny % 2]
                ny += 1
                c16 = rt * 4 + s
                for nb in range(4):
                    c = nc2 % 2
                    nc2 += 1

                    def mm2(e, c=c, s=s, nb=nb):
                        for fc in range(16):
                            ins = e.matmul(py[c][:], hidT[:, fc, s * 128:(s + 1) * 128], w2f[:, fc, nb * 512:(nb + 1) * 512],
                                           start=(fc == 0), stop=(fc == 15))
                        return ins
                    S.op("pe", mm2, reads=[hidT, w2f], writes=[py[c]])
                    S.op("dve", lambda e, c=c, ys=ys, nb=nb, j=j, c16=c16: e.tensor_scalar(
                        out=ys[:, nb * 512:(nb + 1) * 512], in0=py[c][:], scalar1=K.gate[:, j, c16:c16 + 1], scalar2=None, op0=ALU.mult),
                        reads=[py[c], K.gate], writes=[ys])
                S.dma("pool", lambda e, ys=ys, j=j, c16=c16: e.indirect_dma_start(
                    out=delta, out_offset=bass.IndirectOffsetOnAxis(ap=K.idx_d[:, j, c16:c16 + 1], axis=0), in_=ys[:], in_offset=None,
                    compute_op=ALU.add), reads=[ys, K.idx_d], writes=[K.delta_buf])


def op_ple(S, nc, K, L, xdst, xdst_off):
    with ExitStack() as st:
        wpg = sb(nc, st, "pl_wpg", [128, 16, D], BF16)
        wpe = sb(nc, st, "pl_wpe", [128, 2, D], BF16)
        g = sb(nc, st, "pl_g", [128, D], F32)
        xt = [sb(nc, st, "pl_x%d" % i, [128, D], F32) for i in range(2)]
        dl = [sb(nc, st, "pl_d%d" % i, [128, D], F32) for i in range(2)]
        junk = sb(nc, st, "pl_junk", [128, D], F32)
        xn = sb(nc, st, "pl_xn", [128, D], BF16)
        xrT = sb(nc, st, "pl_xrT", [128, 16, 128], BF16)
        pTt = [sb(nc, st, "pl_pT%d" % i, [128, 2, 128], BF16) for i in range(2)]
        pe_sb = sb(nc, st, "pl_pe", [128, D], F32)
        sg = sb(nc, st, "pl_sg", [128, D], F32)
        ss = [sb(nc, st, "pl_ss%d" % i, [128, 4], F32) for i in range(2)]
        pT = ps(nc, st, "pl_pTp", [128, D], BF16)
        pp = [ps(nc, st, "pl_pp%d" % i, [128, 512]) for i in range(2)]
        pg = [ps(nc, st, "pl_pg%d" % i, [128, 512]) for i in range(2)]
        S.dma("sp", lambda e: e.dma_start(out=wpg[:], in_=fm(K.wb_pg)), writes=[wpg])
        S.dma("sp", lambda e: e.dma_start(out=wpe[:], in_=fm(K.wb_pe)), writes=[wpe])
        S.dma("sp", lambda e: e.dma_start(out=g[:], in_=K.gains[L, 2]), writes=[g])
        n = 0
        for tt in range(T // 128):
            i = tt % 2
            t0 = tt * 128
            x, d = xt[i], dl[i]
            S.dma("sp", lambda e, x=x, t0=t0: e.dma_start(out=x[:], in_=K.x1[t0:t0 + 128, :]), writes=[x])
            S.dma("sp", lambda e, d=d, t0=t0: e.dma_start(out=d[:], in_=K.delta_own[L][t0:t0 + 128, :]), writes=[d])
            S.dma("pool", lambda e, i=i, t0=t0: e.dma_start(out=pTt[i][:], in_=K.pT_in[L].rearrange("(kc p) t -> p kc t", p=128)[:, :, t0:t0 + 128]),
                  writes=[pTt[i]])
            S.op("dve", lambda e, x=x, d=d: e.tensor_tensor(out=x[:], in0=x[:], in1=d[:], op=ALU.add), reads=[x, d], writes=[x])
            S.op("act", lambda e, x=x: e.activation(out=junk[:], in_=x[:], func=AF.Square), reads=[x], writes=[junk])
            S.op("dve", lambda e, i=i: e.tensor_reduce(out=ss[i][:, 0:1], in_=junk[:], axis=AX.X, op=ALU.add), reads=[junk], writes=[ss[i]])
            S.op("act", lambda e, i=i: e.activation(out=ss[i][:, 1:2], in_=ss[i][:, 0:1], func=AF.Sqrt, scale=1.0 / D, bias=K.epsc[:, 0:1]),
                 reads=[ss[i], K.epsc], writes=[ss[i]])
            S.op("dve", lambda e, i=i: e.reciprocal(out=ss[i][:, 1:2], in_=ss[i][:, 1:2]), reads=[ss[i]], writes=[ss[i]])
            S.op("dve", lambda e, i=i, x=x: e.tensor_scalar(out=xn[:], in0=x[:], scalar1=ss[i][:, 1:2], scalar2=None, op0=ALU.mult),
                 reads=[x, ss[i]], writes=[xn])

            def tr(e):
                for kc in range(16):
                    ins = e.transpose(out=pT[:, kc * 128:(kc + 1) * 128], in_=xn[:, kc * 128:(kc + 1) * 128], identity=K.identb[:])
                return ins
            S.op("pe", tr, reads=[xn, K.identb], writes=[pT])
            S.op("act", lambda e: e.activation(out=xrT[:], in_=pT[:].rearrange("p (k t) -> p k t", k=16), func=AF.Copy), reads=[pT], writes=[xrT])
            for nb in range(4):
                c = n % 2
                n += 1

                def mm(e, c=c, nb=nb, i=i):
                    for kc in range(2):
                        e.matmul(pp[c][:], pTt[i][:, kc, :], wpe[:, kc, nb * 512:(nb + 1) * 512], start=(kc == 0), stop=(kc == 1))
                    for kc in range(16):
                        ins = e.matmul(pg[c][:], xrT[:, kc, :], wpg[:, kc, nb * 512:(nb + 1) * 512], start=(kc == 0), stop=(kc == 15))
                    return ins
                S.op("pe", mm, reads=[pTt[i], wpe, xrT, wpg], writes=[pp[c], pg[c]])
                S.op("act", lambda e, c=c, nb=nb: e.activation(out=pe_sb[:, nb * 512:(nb + 1) * 512], in_=pp[c][:], func=AF.Copy),
                     reads=[pp[c]], writes=[pe_sb])
                S.op("act", lambda e, c=c, nb=nb: e.activation(out=sg[:, nb * 512:(nb + 1) * 512], in_=pg[c][:], func=AF.Sigmoid),
                     reads=[pg[c]], writes=[sg])
            S.op("act", lambda e: e.activation(out=junk[:], in_=pe_sb[:], func=AF.Square), reads=[pe_sb], writes=[junk])
            S.op("dve", lambda e, i=i: e.tensor_reduce(out=ss[i][:, 2:3], in_=junk[:], axis=AX.X, op=ALU.add), reads=[junk], writes=[ss[i]])
            S.op("act", lambda e, i=i: e.activation(out=ss[i][:, 3:4], in_=ss[i][:, 2:3], func=AF.Sqrt, scale=1.0 / D, bias=K.epsc[:, 0:1]),
                 reads=[ss[i], K.epsc], writes=[ss[i]])
            S.op("dve", lambda e, i=i: e.reciprocal(out=ss[i][:, 3:4], in_=ss[i][:, 3:4]), reads=[ss[i]], writes=[ss[i]])
            S.op("dve", lambda e, i=i: e.scalar_tensor_tensor(out=pe_sb[:], in0=pe_sb[:], scalar=ss[i][:, 3:4], in1=g[:], op0=ALU.mult, op1=ALU.mult),
                 reads=[pe_sb, ss[i], g], writes=[pe_sb])
            S.op("dve", lambda e: e.tensor_tensor(out=pe_sb[:], in0=pe_sb[:], in1=sg[:], op=ALU.mult), reads=[pe_sb, sg], writes=[pe_sb])
            S.op("dve", lambda e, x=x: e.tensor_tensor(out=x[:], in0=x[:], in1=pe_sb[:], op=ALU.add), reads=[x, pe_sb], writes=[x])
            S.dma("pool", lambda e, x=x, t0=t0: e.dma_start(out=xdst[xdst_off + t0:xdst_off + t0 + 128, :], in_=x[:]), reads=[x])
        S.barrier()


def op_halo(S, nc, K, xext):
    with ExitStack() as st:
        t = [sb(nc, st, "hl_t%d" % i, [128, D], F32) for i in range(2)]
        for k in range(4):
            src0 = (HALO + T - 256 + k * 128) if k < 2 else (HALO + (k - 2) * 128)
            b = t[k % 2]
            S.dma("sp", lambda e, b=b, src0=src0: e.dma_start(out=b[:], in_=xext[src0:src0 + 128, :]), writes=[b])
            S.dma("sp", lambda e, b=b, k=k: e.dma_start(out=K.hsend[k * 128:(k + 1) * 128, :], in_=b[:]), reads=[b])
        S.barrier()
        for k in range(4):
            S.coll(lambda e, k=k: e.collective_compute("AllGather", ALU.bypass, replica_groups=K.groups,
                                                       ins=[K.hsend[k * 128:(k + 1) * 128, :]], outs=[K.hall[k * 512:(k + 1) * 512, :]]))
        S.barrier()
        for k in range(4):
            b = t[k % 2]
            S.dma("pool", lambda e, b=b, k=k: e.indirect_dma_start(
                out=b[:], out_offset=None, in_=K.hall, in_offset=bass.IndirectOffsetOnAxis(ap=K.hidx[:, k:k + 1], axis=0)),
                reads=[K.hidx], writes=[b])
            S.op("dve", lambda e, b=b, k=k: e.tensor_scalar(out=b[:], in0=b[:], scalar1=K.hmask[:, k // 2:k // 2 + 1], scalar2=None, op0=ALU.mult),
                 reads=[b, K.hmask], writes=[b])
            dst0 = k * 128 if k < 2 else HALO + T + (k - 2) * 128
            S.dma("sp", lambda e, b=b, dst0=dst0: e.dma_start(out=xext[dst0:dst0 + 128, :], in_=b[:]), reads=[b])
        S.barrier()


def build_program(dbg=(), nlayers=NL, nli=NL):
    nc = bass.Bass("TRN2", target_bir_lowering=False)
    K = Ctx()
    for _b in _CAST_BUF:
        _b.w, _b.r = None, []

    def din(name, shape, dt=F32):
        return nc.dram_tensor(name, list(shape), dt, kind="ExternalInput").ap()

    def dscr(name, shape, dt):
        kind = "ExternalOutput" if name in dbg else "Internal"
        return nc.dram_tensor(name, list(shape), dt, kind=kind).ap()

    xe0 = din("xe0", [TE, D])
    w_in = din("w_in", [nli, D, NIN])
    w_ao = din("w_att_o", [nli, 1024, D])
    w_co = din("w_conv_o", [nli, 1024, D])
    w_out = din("w_out", [nli, D, D])
    w_pg = din("w_pg", [nli, D, D])
    w_pe = din("w_pe", [nli, 256, D])
    K.w_router = din("w_router", [nli, D, 16])
    w1s = din("w1s", [nli, 4, D, D])
    w3s = din("w3s", [nli, 4, D, D])
    w2s = din("w2s", [nli, 4, D, D])
    K.pT_in = din("pT_in", [nli, 256, T])
    K.gains = din("gains", [nli, 3, 128, D])
    qkg = din("qkg_in", [128, nli, 2])
    wdw = din("wdw_in", [128, nli, 8, 31])
    cvp = din("cvp_in", [128, nli, 3, 8])
    K.att_tab = din("att_tab", [nli, 16, 128, 3, ATT_COLS])
    identb_d = din("identb_in", [128, 128])
    blk64_d = din("blk64_in", [128, 128])
    tri_d = din("tri_in", [128, 128])
    iota_d = din("iota_in", [128, CAP])
    K.rtab_in = din("rtab_in", [128, 4, 128, 6])
    ridx_d = din("ridx_in", [128, 4], I32)
    hidx_d = din("hidx_in", [128, 4], I32)
    hmask_d = din("hmask_in", [128, 2])
    y = nc.dram_tensor("y", [T, D], F32, kind="ExternalOutput").ap()

    K.wb_in = dscr("wb_in", [D, NIN], BF16)
    K.wb_ao = dscr("wb_ao", [1024, D], BF16)
    K.wb_co = dscr("wb_co", [1024, D], BF16)
    K.wb_out = dscr("wb_out", [D, D], BF16)
    K.wb_pg = dscr("wb_pg", [D, D], BF16)
    K.wb_pe = dscr("wb_pe", [256, D], BF16)
    K.wb_1 = dscr("wb_1", [4, D, D], BF16)
    K.wb_3 = dscr("wb_3", [4, D, D], BF16)
    K.wb_2 = dscr("wb_2", [4, D, D], BF16)
    K.hT = dscr("hT", [D, TE], BF16)
    K.qT = dscr("qT", [1024, TE], BF16)
    K.kT = dscr("kT", [1024, TE], BF16)
    K.v = dscr("v", [TE, 1024], BF16)
    K.gluT = dscr("gluT", [1024, TE], F32)
    K.gaT = dscr("gaT", [D, TE], BF16)
    K.gcT = dscr("gcT", [D, TE], BF16)
    K.attT = dscr("attT", [1024, T], BF16)
    K.convT = dscr("convT", [1024, T], BF16)
    K.mT = dscr("mT", [D, T], BF16)
    x1s = [dscr("x1_%d" % l, [T, D], F32) for l in range(NL)]
    affs = [dscr("affT_own%d" % l, [512, 128], F32) for l in range(NL)]
    xext1 = dscr("xext1", [TE, D], F32)
    K.h2own = dscr("h2own", [T, D], BF16)
    K.affT_all = dscr("affT_all", [2048, 128], F32)
    K.h2_all = [dscr("h2_all%d" % l, [4 * T, D], BF16) for l in range(NL)]
    K.delta = [dscr("delta%d" % l, [4 * T, D], F32) for l in range(NL)]
    K.delta_own = [dscr("delta_own%d" % l, [T, D], F32) for l in range(NL)]
    K.hsend = dscr("hsend", [512, D], F32)
    K.hall = dscr("hall", [2048, D], F32)
    K.groups = [[0, 1, 2, 3], [4, 5, 6, 7]]
    K.delta_buf = Buf(None)

    with ExitStack() as st:
        S = Sched(nc)
        K.identb = sb(nc, st, "identb", [128, 128], BF16)
        K.identf = sb(nc, st, "identf", [128, 128], F32)
        K.blk64 = sb(nc, st, "blk64", [128, 128], F32)
        K.tri = sb(nc, st, "tri", [128, 128], F32)
        K.ones_f = sb(nc, st, "ones_f", [128, 128], F32)
        K.ones_b = sb(nc, st, "ones_b", [128, 128], BF16)
        K.qkg_t = sb(nc, st, "qkg_t", [128, nli, 2], F32)
        K.qg = sb(nc, st, "qg", [128, 1], F32)
        K.kg = sb(nc, st, "kg", [128, 1], F32)
        K.wdw = sb(nc, st, "wdw", [128, nli, 8, 31], F32)
        K.cvp = sb(nc, st, "cvp", [128, nli, 3, 8], F32)
        K.ridx = sb(nc, st, "ridx", [128, 4], I32)
        K.hidx = sb(nc, st, "hidx", [128, 4], I32)
        K.hmask = sb(nc, st, "hmask", [128, 2], F32)
        K.epsc = sb(nc, st, "epsc", [128, 1], F32)
        S.dma("pool", lambda e: e.dma_start(out=K.identb[:], in_=identb_d), writes=[K.identb])
        S.dma("sp", lambda e: e.dma_start(out=K.identf[:], in_=identb_d), writes=[K.identf])
        S.dma("sp", lambda e: e.dma_start(out=K.blk64[:], in_=blk64_d), writes=[K.blk64])
        S.dma("sp", lambda e: e.dma_start(out=K.tri[:], in_=tri_d), writes=[K.tri])
        S.dma("sp", lambda e: e.dma_start(out=K.qkg_t[:], in_=qkg), writes=[K.qkg_t])
        S.dma("sp", lambda e: e.dma_start(out=K.wdw[:], in_=wdw), writes=[K.wdw])
        S.dma("sp", lambda e: e.dma_start(out=K.cvp[:], in_=cvp), writes=[K.cvp])
        S.dma("sp", lambda e: e.dma_start(out=K.ridx[:], in_=ridx_d), writes=[K.ridx])
        S.dma("sp", lambda e: e.dma_start(out=K.hidx[:], in_=hidx_d), writes=[K.hidx])
        S.dma("sp", lambda e: e.dma_start(out=K.hmask[:], in_=hmask_d), writes=[K.hmask])
        S.op("dve", lambda e: e.memset(K.epsc[:], EPS), writes=[K.epsc])
        S.op("dve", lambda e: e.memset(K.ones_f[:], 1.0), writes=[K.ones_f])
        S.op("dve", lambda e: e.memset(K.ones_b[:], 1.0), writes=[K.ones_b])
        S.barrier()

        for L in range(nlayers):
            last = L == nlayers - 1
            xin = xe0 if L == 0 else xext1
            K.x1 = x1s[L]
            K.affT_own = affs[L]
            S.op("dve", lambda e, L=L: e.tensor_scalar(out=K.qg[:], in0=K.qkg_t[:, L, 0:1], scalar1=0.125, scalar2=None, op0=ALU.mult),
                 reads=[K.qkg_t], writes=[K.qg])
            S.op("dve", lambda e, L=L: e.tensor_copy(out=K.kg[:], in_=K.qkg_t[:, L, 1:2]), reads=[K.qkg_t], writes=[K.kg])
            op_cast(S, w_in[L], K.wb_in, D, NIN)
            op_cast(S, w_ao[L], K.wb_ao, 1024, D)
            op_cast(S, w_co[L], K.wb_co, 1024, D)
            op_cast(S, w_out[L], K.wb_out, D, D)
            op_cast(S, w_pg[L], K.wb_pg, D, D)
            op_cast(S, w_pe[L], K.wb_pe, 256, D)
            for j in range(4):
                op_cast(S, w1s[L, j], K.wb_1[j], D, D)
                op_cast(S, w3s[L, j], K.wb_3[j], D, D)
                op_cast(S, w2s[L, j], K.wb_2[j], D, D)
            S.barrier()
            op_norm_T(S, nc, K, xin, TE, K.gains[L, 0], K.hT, "n1")
            op_inproj(S, nc, K, L)
            op_attention(S, nc, K, L)
            op_conv(S, nc, K, L)
            op_mix(S, nc, K, L)
            op_resid_linear(S, nc, K, K.mT, K.wb_out, 16, xin, HALO, K.x1, "ro")
            op_post(S, nc, K, L)
            S.coll(lambda e: e.collective_compute("AllGather", ALU.bypass, replica_groups=K.groups, ins=[K.affT_own], outs=[K.affT_all]))
            for k in range(16):
                S.coll(lambda e, L=L, k=k: e.collective_compute("AllGather", ALU.bypass, replica_groups=K.groups,
                                                                ins=[K.h2own[k * 256:(k + 1) * 256, :]], outs=[K.h2_all[L][k * 1024:(k + 1) * 1024, :]]))
            S.barrier()
            with ExitStack() as st2:
                K.idx = sb(nc, st2, "idx", [128, 4, 16], I32)
                K.idx_d = sb(nc, st2, "idx_d", [128, 4, 16], I32)
                K.gate = sb(nc, st2, "gate", [128, 4, 16], F32)
                K.iota_s = sb(nc, st2, "iota_s", [128, CAP], F32)
                S.dma("sp", lambda e: e.dma_start(out=K.iota_s[:], in_=iota_d), writes=[K.iota_s])
                with ExitStack() as st3:
                    op_zero_delta(S, nc, K, L, st3)
                    op_route(S, nc, K, L, st3)
                    S.barrier()
                with ExitStack() as st3:
                    op_expert(S, nc, K, L, st3)
                    S.barrier()
            for k in range(32):
                S.coll(lambda e, L=L, k=k: e.collective_compute("ReduceScatter", ALU.add, replica_groups=K.groups,
                                                                ins=[K.delta[L][k * 512:(k + 1) * 512, :]], outs=[K.delta_own[L][k * 128:(k + 1) * 128, :]]))
            S.barrier()
            if last:
                op_ple(S, nc, K, L, y, 0)
            else:
                op_ple(S, nc, K, L, xext1, HALO)
                op_halo(S, nc, K, xext1)
        S.barrier()
        S.close()
    return nc


def _att_tables(rpb):
    Lr = rpb.shape[0]
    tabs = np.full((3, Lr, 16, 128, ATT_COLS), NEG, np.float32)
    kr = np.arange(128) // 64
    kc = np.arange(128) % 64
    for slot in range(3):
        o = 0
        for ci, (a, b) in enumerate(ATT_RNG):
            rel = 2 * ci - 4 + kr
            for i in range(a, b + 1):
                if (slot == 0 and i < 4) or (slot == 2 and i >= 4):
                    ok_r = (rel >= 0) & (rel <= 7)
                else:
                    ok_r = (rel >= i - 4) & (rel <= i + 3)
                dr = rel - i
                for qc in range(64):
                    cs = min(max(qc - 8, 0), 48)
                    ok = ok_r & (kc >= cs) & (kc < cs + 16)
                    idx = np.nonzero(ok)[0]
                    if idx.size == 0:
                        continue
                    col = o + (i - a) * 64 + qc
                    tabs[slot][:, :, idx, col] = rpb[:, :, dr[idx] + 7, kc[idx] - qc + 15]
            o += (b - a + 1) * 64
    return tabs


def _shared(inp):
    g = np.stack([inp["norm1_g"], inp["norm2_g"], inp["pe_norm_g"]], axis=1)
    gains = np.ascontiguousarray(np.broadcast_to(g[:, :, None, :], (NL, 3, 128, D))).astype(np.float32)
    qkg = np.zeros((128, NL, 2), np.float32)
    for l in range(NL):
        qkg[:, l, 0] = np.tile(inp["q_norm_g"][l], 2)
        qkg[:, l, 1] = np.tile(inp["k_norm_g"][l], 2)
    wdw = np.ascontiguousarray(inp["w_dw"].reshape(NL, 31, 8, 128).transpose(3, 0, 2, 1)).astype(np.float32)
    cvp = np.stack([inp["b_dw"], inp["conv_ln_g"], inp["conv_ln_b"]], axis=1)
    cvp = np.ascontiguousarray(cvp.reshape(NL, 3, 8, 128).transpose(3, 0, 1, 2)).astype(np.float32)
    blk64 = np.zeros((128, 128), np.float32)
    blk64[:64, :64] = 1.0
    blk64[64:, 64:] = 1.0
    tri = np.triu(np.ones((128, 128), np.float32), 1)
    iota = np.ascontiguousarray(np.broadcast_to(np.arange(CAP, dtype=np.float32)[None, :], (128, CAP)))
    rtab = np.zeros((128, 4, 128, 6), np.float32)
    pa = np.arange(128)
    rr, pp = pa // 32, pa % 32
    g_h2 = (pp // 2) * 1024 + rr * 256 + (pp % 2) * 128
    g_dl = pp * 512 + rr * 128
    rtab[:, :, :, 0] = g_h2.astype(np.float32)[:, None, None]
    rtab[:, :, :, 1] = g_dl.astype(np.float32)[:, None, None]
    rtab[:, :, :, 2] = np.arange(128, dtype=np.float32)[None, None, :]
    common = {
        "w_in": inp["w_in"], "w_att_o": inp["w_att_o"], "w_conv_o": inp["w_conv_o"], "w_out": inp["w_out"],
        "w_pg": inp["w_pg"], "w_pe": inp["w_pe"], "w_router": inp["w_router"],
        "gains": gains, "qkg_in": qkg, "wdw_in": wdw, "cvp_in": cvp,
        "identb_in": np.eye(128, dtype=np.float32), "blk64_in": blk64, "tri_in": tri, "iota_in": iota, "rtab_in": rtab,
    }
    return {"common": common, "tabs": _att_tables(np.asarray(inp["rpb"], np.float32))}


def _prep_core(c, inp, shared):
    b, q = c // 4, c % 4
    x = inp["x"]
    xe = np.zeros((TE, D), np.float32)
    lo = q * T - HALO
    hi = (q + 1) * T + HALO
    s0, s1 = max(lo, 0), min(hi, 4 * T)
    xe[s0 - lo:s1 - lo] = x[b, s0:s1]
    tabs = shared["tabs"]
    sel = [0 if q == 0 else 1, 1, 2 if q == 3 else 1]
    att_tab = np.stack([tabs[s] for s in sel], axis=3)
    m = dict(shared["common"])
    m["xe0"] = xe
    m["att_tab"] = np.ascontiguousarray(att_tab)
    m["w1s"] = np.ascontiguousarray(inp["w1"][:, 4 * q:4 * q + 4])
    m["w3s"] = np.ascontiguousarray(inp["w3"][:, 4 * q:4 * q + 4])
    m["w2s"] = np.ascontiguousarray(inp["w2"][:, 4 * q:4 * q + 4])
    m["pT_in"] = np.ascontiguousarray(inp["p"][:, b, q * T:(q + 1) * T, :].transpose(0, 2, 1))
    pidx = np.arange(128)
    r, pp = pidx // 32, pidx % 32
    ridx = np.zeros((128, 4), np.int32)
    for j in range(4):
        ridx[:, j] = (r * 16 + 4 * q + j) * 32 + pp
    hidx = np.zeros((128, 4), np.int32)
    hmask = np.zeros((128, 2), np.float32)
    if q > 0:
        hmask[:, 0] = 1.0
        for k in range(2):
            hidx[:, k] = k * 512 + (q - 1) * 128 + pidx
    if q < 3:
        hmask[:, 1] = 1.0
        for k in range(2):
            hidx[:, 2 + k] = (2 + k) * 512 + (q + 1) * 128 + pidx
    m["ridx_in"] = ridx
    m["hidx_in"] = hidx
    m["hmask_in"] = hmask
    return m


_LEAD = ("w_in", "w_att_o", "w_conv_o", "w_out", "w_pg", "w_pe", "w_router", "w1s", "w3s", "w2s", "pT_in", "gains", "att_tab")
_SECOND = ("qkg_in", "wdw_in", "cvp_in")
FUSED = True


def _layer_slice(m, L):
    o = dict(m)
    for k in _LEAD:
        o[k] = np.ascontiguousarray(m[k][L:L + 1])
    for k in _SECOND:
        o[k] = np.ascontiguousarray(m[k][:, L:L + 1])
    return o


def _gather_out(res):
    out = np.zeros((2, 4 * T, D), np.float32)
    for c in range(NCORES):
        out[c // 4, (c % 4) * T:(c % 4 + 1) * T] = res.results[c]["y"]
    return out


def kernel(**inputs):
    inp = {k: np.asarray(v) for k, v in inputs.items()}
    shared = _shared(inp)
    if FUSED:
        nc = build_program()
        in_maps = [_prep_core(c, inp, shared) for c in range(NCORES)]
        return _gather_out(run_bass_kernel_spmd(nc, in_maps, core_ids=list(range(NCORES))))
    x = inp["x"]
    for L in range(NL):
        cur = dict(inp)
        cur["x"] = x
        nc = build_program(nlayers=1, nli=1)
        in_maps = [_layer_slice(_prep_core(c, cur, shared), L) for c in range(NCORES)]
        x = _gather_out(run_bass_kernel_spmd(nc, in_maps, core_ids=list(range(NCORES))))
    return x
```
